# Optimizing a Trainium2 kernel written in Bass

```python
import math
import jax, jax.numpy as jnp
from jax import lax
import numpy as np

D_MODEL = 1024
BATCH = 8
SEQ = 4096
DEPTH = 1

CHUNK = 64
MEM_LEN = 256
LN_EPS = 1e-5
SB_HEADS = 16
SB_HEAD_DIM = 64
SB_WIDTH = SB_HEADS * SB_HEAD_DIM
SB_BLOCK = 128
SSD_EXPAND = 2
SSD_INNER = SSD_EXPAND * D_MODEL
SSD_HEAD_DIM = 64
SSD_HEADS = SSD_INNER // SSD_HEAD_DIM
SSD_GROUPS = 4
SSD_HEADS_PER_GROUP = SSD_HEADS // SSD_GROUPS
SSD_STATE = 128
SSD_CONV = 4
SSD_CONV_CH = SSD_INNER + 2 * SSD_GROUPS * SSD_STATE
N_BRANCH = 2
IN_WIDTH = 3 * SB_WIDTH + SSD_INNER + SSD_CONV_CH + SSD_HEADS + N_BRANCH * D_MODEL
X_HEADS = 4
X_HEAD_DIM = D_MODEL // X_HEADS
N_EXPERTS = 32
TOP_K = 4
D_EXPERT = D_MODEL
SWIGLU_LIMIT = 7.0
SWIGLU_ALPHA = 1.702
EXPERT_BLOCK = 128
DN_ALPHA = (2 * DEPTH) ** 0.25
DN_BETA = (8 * DEPTH) ** -0.25

kernel_name = 'hybrid_stickbreak_ssd_moe_deepnorm'


def layer_norm(x, g, b):
    xf = x.astype(jnp.float32)
    mu = jnp.mean(xf, axis=-1, keepdims=True)
    var = jnp.mean(jnp.square(xf - mu), axis=-1, keepdims=True)
    return ((xf - mu) * lax.rsqrt(var + LN_EPS)).astype(x.dtype) * g + b


def stick_breaking_attention(q, k, v):
    S = q.shape[1]
    scale = SB_HEAD_DIM ** -0.5
    outs = []
    for start in range(0, S, SB_BLOCK):
        end = start + SB_BLOCK
        qb = q[:, start:end]
        kb = k[:, :end]
        vb = v[:, :end]
        z = jnp.einsum('bqhd,bkhd->bhqk', qb, kb, preferred_element_type=jnp.float32) * scale
        t_idx = start + jnp.arange(SB_BLOCK)
        s_idx = jnp.arange(end)
        mask = s_idx[None, :] < t_idx[:, None]
        log_1m = jnp.where(mask, jax.nn.log_sigmoid(-z), 0.0)
        rev = lax.cumsum(log_1m, axis=3, reverse=True)
        after = jnp.pad(rev[..., 1:], ((0, 0), (0, 0), (0, 0), (0, 1)))
        w = jnp.where(mask, jnp.exp(jax.nn.log_sigmoid(z) + after), 0.0)
        outs.append(jnp.einsum('bhqk,bkhd->bqhd', w.astype(v.dtype), vb))
    return jnp.concatenate(outs, axis=1)


def causal_depthwise_conv(u, w, b):
    out = lax.conv_general_dilated(
        u, w[:, None, :].astype(u.dtype), window_strides=(1,), padding=[(SSD_CONV - 1, 0)],
        dimension_numbers=('NWC', 'WIO', 'NWC'), feature_group_count=u.shape[-1])
    return out + b


def ssd_chunked_scan(xdt, da, bm, cm):
    bsz, S = xdt.shape[:2]
    nc = S // CHUNK

    def to_chunks(u):
        u = u.reshape((bsz, nc, CHUNK) + u.shape[2:])
        return jnp.moveaxis(u, 1, 0)

    causal = jnp.tril(jnp.ones((CHUNK, CHUNK), dtype=bool))[None, :, :, None, None]

    def step(h, inp):
        x_c, da_c, b_c, c_c = inp
        cs = jnp.cumsum(da_c, axis=1)
        diff = cs[:, :, None] - cs[:, None, :]
        seg = jnp.exp(jnp.where(causal, diff, -jnp.inf))
        cb = jnp.einsum('btgn,bsgn->btsg', c_c, b_c)
        y = jnp.einsum('btsg,btsgr,bsgrp->btgrp', cb, seg, x_c)
        y = y + jnp.einsum('btgn,bgrpn->btgrp', c_c, h) * jnp.exp(cs)[..., None]
        decay_end = jnp.exp(cs[:, -1:] - cs)
        h = h * jnp.exp(cs[:, -1])[..., None, None] + jnp.einsum('bsgn,bsgr,bsgrp->bgrpn', b_c, decay_end, x_c)
        return h, y

    h0 = jnp.zeros((bsz, SSD_GROUPS, SSD_HEADS_PER_GROUP, SSD_HEAD_DIM, SSD_STATE), jnp.float32)
    _, ys = lax.scan(step, h0, (to_chunks(xdt), to_chunks(da), to_chunks(bm), to_chunks(cm)))
    return jnp.moveaxis(ys, 0, 1).reshape(xdt.shape)


def hybrid_mixer(h, w_in, b_branch_gate, conv_w, conv_b, dt_bias, a_log, d_skip, ssd_norm_g,
                 w_sb, w_ssd, w_mix_out):
    bsz, S, _ = h.shape
    splits = list(np.cumsum([SB_WIDTH, SB_WIDTH, SB_WIDTH, SSD_INNER, SSD_CONV_CH, SSD_HEADS]))
    q, k, v, z, xbc, dt_raw, gate_logits = jnp.split(h @ w_in, splits, axis=-1)
    hs = (bsz, S, SB_HEADS, SB_HEAD_DIM)
    o_sb = stick_breaking_attention(q.reshape(hs), k.reshape(hs), v.reshape(hs)).reshape(bsz, S, SB_WIDTH)
    xbc = jax.nn.silu(causal_depthwise_conv(xbc, conv_w, conv_b))
    xs, bm, cm = jnp.split(xbc, [SSD_INNER, SSD_INNER + SSD_GROUPS * SSD_STATE], axis=-1)
    xh = xs.reshape(bsz, S, SSD_GROUPS, SSD_HEADS_PER_GROUP, SSD_HEAD_DIM).astype(jnp.float32)
    bm = bm.reshape(bsz, S, SSD_GROUPS, SSD_STATE).astype(jnp.float32)
    cm = cm.reshape(bsz, S, SSD_GROUPS, SSD_STATE).astype(jnp.float32)
    dt = jax.nn.softplus(dt_raw.astype(jnp.float32) + dt_bias.astype(jnp.float32))
    dt = dt.reshape(bsz, S, SSD_GROUPS, SSD_HEADS_PER_GROUP)
    a = -jnp.exp(a_log.astype(jnp.float32)).reshape(SSD_GROUPS, SSD_HEADS_PER_GROUP)
    y = ssd_chunked_scan(xh * dt[..., None], dt * a, bm, cm)
    y = y + d_skip.astype(jnp.float32).reshape(SSD_GROUPS, SSD_HEADS_PER_GROUP)[..., None] * xh
    y = y.reshape(bsz, S, SSD_INNER) * jax.nn.silu(z.astype(jnp.float32))
    yg = y.reshape(bsz, S, SSD_GROUPS, SSD_INNER // SSD_GROUPS)
    yg = yg * lax.rsqrt(jnp.mean(jnp.square(yg), axis=-1, keepdims=True) + LN_EPS)
    o_ssd = yg.reshape(bsz, S, SSD_INNER).astype(h.dtype) * ssd_norm_g
    gates = jax.nn.sigmoid(gate_logits.reshape(bsz, S, N_BRANCH, D_MODEL) + b_branch_gate)
    merged = gates[:, :, 0] * (o_sb @ w_sb) + gates[:, :, 1] * (o_ssd @ w_ssd)
    return merged @ w_mix_out


def memory_cross_attention(h, mem, w_xq, w_xk, w_xv, w_xo):
    bsz, S, _ = h.shape
    q = (h @ w_xq).reshape(bsz, S, X_HEADS, X_HEAD_DIM)
    k = (mem @ w_xk).reshape(bsz, mem.shape[1], X_HEADS, X_HEAD_DIM)
    v = (mem @ w_xv).reshape(bsz, mem.shape[1], X_HEADS, X_HEAD_DIM)
    s = jnp.einsum('bqhd,bmhd->bhqm', q, k, preferred_element_type=jnp.float32) * (X_HEAD_DIM ** -0.5)
    p = jax.nn.softmax(s, axis=-1).astype(v.dtype)
    o = jnp.einsum('bhqm,bmhd->bqhd', p, v).reshape(bsz, S, D_MODEL)
    return o @ w_xo


def moe_ffn(h, w_router, b_router, w_e_gate, b_e_gate, w_e_up, b_e_up, w_e_down, b_e_down):
    bsz, S, D = h.shape
    T = bsz * S
    TK = T * TOP_K
    xt = h.reshape(T, D)
    logits = (xt @ w_router).astype(jnp.float32) + b_router.astype(jnp.float32)
    top_vals, top_idx = lax.top_k(logits, TOP_K)
    probs = jax.nn.softmax(top_vals, axis=-1)
    flat_e = top_idx.reshape(TK)
    flat_tok = jnp.arange(TK, dtype=jnp.int32) // TOP_K
    flat_p = probs.reshape(TK)
    order = jnp.argsort(flat_e)
    se = flat_e[order]
    counts = jnp.bincount(flat_e, length=N_EXPERTS)
    padded = (counts + EXPERT_BLOCK - 1) // EXPERT_BLOCK * EXPERT_BLOCK
    start_sorted = jnp.cumsum(counts) - counts
    end_padded = jnp.cumsum(padded)
    start_padded = end_padded - padded
    dest = start_padded[se] + jnp.arange(TK, dtype=jnp.int32) - start_sorted[se]
    n_blocks = -(-(TK + N_EXPERTS * (EXPERT_BLOCK - 1)) // EXPERT_BLOCK)
    n_rows = n_blocks * EXPERT_BLOCK
    row_tok = jnp.full((n_rows,), T, dtype=jnp.int32).at[dest].set(flat_tok[order])
    row_p = jnp.zeros((n_rows,), jnp.float32).at[dest].set(flat_p[order])
    block_e = jnp.minimum(jnp.searchsorted(end_padded, jnp.arange(n_blocks) * EXPERT_BLOCK, side='right'),
                          N_EXPERTS - 1)
    x_rows = jnp.concatenate([xt, jnp.zeros((1, D), xt.dtype)], axis=0)[row_tok]
    x_rows = x_rows.reshape(n_blocks, EXPERT_BLOCK, D)

    def expert_block(args):
        xb, e = args
        g = jnp.minimum(xb @ w_e_gate[e] + b_e_gate[e], SWIGLU_LIMIT)
        u = jnp.clip(xb @ w_e_up[e] + b_e_up[e], -SWIGLU_LIMIT, SWIGLU_LIMIT)
        act = (u + 1.0) * g * jax.nn.sigmoid(SWIGLU_ALPHA * g)
        return act @ w_e_down[e] + b_e_down[e]

    y_rows = lax.map(expert_block, (x_rows, block_e)).reshape(n_rows, D)
    y = jax.ops.segment_sum(y_rows * row_p[:, None].astype(y_rows.dtype), row_tok, num_segments=T + 1)[:T]
    return y.reshape(bsz, S, D)


def setup_inputs(seed: int = 0) -> dict:
    key = jax.random.key(seed)
    keys = iter(jax.random.split(key, 40))

    def nrm(shape, scale):
        return jax.random.normal(next(keys), shape, jnp.float32) * scale

    L = DEPTH
    dt0 = jnp.exp(jax.random.uniform(next(keys), (L, SSD_HEADS), jnp.float32, math.log(1e-3), math.log(1e-1)))
    dt_bias = dt0 + jnp.log(-jnp.expm1(-dt0))
    a_log = jnp.log(jax.random.uniform(next(keys), (L, SSD_HEADS), jnp.float32, 1.0, 16.0))
    return {
        'x': nrm((BATCH, SEQ, D_MODEL), 1.0),
        'mem': nrm((BATCH, MEM_LEN, D_MODEL), 1.0),
        'ln_in_g': 1.0 + nrm((D_MODEL,), 0.02),
        'ln_in_b': nrm((D_MODEL,), 0.02),
        'w_in': nrm((L, D_MODEL, IN_WIDTH), D_MODEL ** -0.5),
        'b_branch_gate': nrm((L, N_BRANCH, D_MODEL), 0.02),
        'conv_w': nrm((L, SSD_CONV, SSD_CONV_CH), SSD_CONV ** -0.5),
        'conv_b': nrm((L, SSD_CONV_CH), 0.02),
        'dt_bias': dt_bias,
        'a_log': a_log,
        'd_skip': 1.0 + nrm((L, SSD_HEADS), 0.1),
        'ssd_norm_g': 1.0 + nrm((L, SSD_INNER), 0.02),
        'w_sb': nrm((L, SB_WIDTH, D_MODEL), SB_WIDTH ** -0.5),
        'w_ssd': nrm((L, SSD_INNER, D_MODEL), SSD_INNER ** -0.5),
        'w_mix_out': nrm((L, D_MODEL, D_MODEL), D_MODEL ** -0.5 * DN_BETA),
        'ln1_g': 1.0 + nrm((L, D_MODEL), 0.02),
        'ln1_b': nrm((L, D_MODEL), 0.02),
        'w_xq': nrm((L, D_MODEL, D_MODEL), D_MODEL ** -0.5),
        'w_xk': nrm((L, D_MODEL, D_MODEL), D_MODEL ** -0.5),
        'w_xv': nrm((L, D_MODEL, D_MODEL), D_MODEL ** -0.5 * DN_BETA),
        'w_xo': nrm((L, D_MODEL, D_MODEL), D_MODEL ** -0.5 * DN_BETA),
        'ln2_g': 1.0 + nrm((L, D_MODEL), 0.02),
        'ln2_b': nrm((L, D_MODEL), 0.02),
        'w_router': nrm((L, D_MODEL, N_EXPERTS), D_MODEL ** -0.5),
        'b_router': nrm((L, N_EXPERTS), 0.01),
        'w_e_gate': nrm((L, N_EXPERTS, D_MODEL, D_EXPERT), D_MODEL ** -0.5),
        'b_e_gate': nrm((L, N_EXPERTS, D_EXPERT), 0.01),
        'w_e_up': nrm((L, N_EXPERTS, D_MODEL, D_EXPERT), D_MODEL ** -0.5),
        'b_e_up': nrm((L, N_EXPERTS, D_EXPERT), 0.01),
        'w_e_down': nrm((L, N_EXPERTS, D_EXPERT, D_MODEL), D_EXPERT ** -0.5 * DN_BETA),
        'b_e_down': nrm((L, N_EXPERTS, D_MODEL), 0.01),
        'ln3_g': 1.0 + nrm((L, D_MODEL), 0.02),
        'ln3_b': nrm((L, D_MODEL), 0.02),
    }


def reference(x, mem, ln_in_g, ln_in_b, w_in, b_branch_gate, conv_w, conv_b, dt_bias, a_log, d_skip,
              ssd_norm_g, w_sb, w_ssd, w_mix_out, ln1_g, ln1_b, w_xq, w_xk, w_xv, w_xo, ln2_g, ln2_b,
              w_router, b_router, w_e_gate, b_e_gate, w_e_up, b_e_up, w_e_down, b_e_down, ln3_g, ln3_b):
    h = layer_norm(x, ln_in_g, ln_in_b)
    for l in range(DEPTH):
        mix = hybrid_mixer(h, w_in[l], b_branch_gate[l], conv_w[l], conv_b[l], dt_bias[l], a_log[l],
                           d_skip[l], ssd_norm_g[l], w_sb[l], w_ssd[l], w_mix_out[l])
        h = layer_norm(DN_ALPHA * h + mix, ln1_g[l], ln1_b[l])
        xa = memory_cross_attention(h, mem, w_xq[l], w_xk[l], w_xv[l], w_xo[l])
        h = layer_norm(DN_ALPHA * h + xa, ln2_g[l], ln2_b[l])
        ff = moe_ffn(h, w_router[l], b_router[l], w_e_gate[l], b_e_gate[l], w_e_up[l], b_e_up[l],
                     w_e_down[l], b_e_down[l])
        h = layer_norm(DN_ALPHA * h + ff, ln3_g[l], ln3_b[l])
    return h
```

```python
import numpy as np
import ml_dtypes
from contextlib import ExitStack
import concourse.bass as bass
import concourse.mybir as mybir
from concourse.bass_utils import run_bass_kernel_spmd

F32 = mybir.dt.float32
BF16 = mybir.dt.bfloat16
AF = mybir.ActivationFunctionType
ALU = mybir.AluOpType
AX = mybir.AxisListType

S = 4096
D = 1024
NT = S // 128
MEM = 256
NE = 32
ALPHA = 2.0 ** 0.25
EPS = 1e-5
IN_W = 10272

ENGS = ['pe', 'act', 'dve', 'pool', 'sp']
NDS = 8


class Prog:
    def __init__(self, nc, stack):
        self.nc = nc
        self.sem = {e: stack.enter_context(nc.semaphore('s_' + e)) for e in ENGS}
        self.dsem = {q: [stack.enter_context(nc.semaphore('d_%s%d' % (q, i))) for i in range(NDS)]
                     for q in ('sp', 'act', 'pool')}
        self.cnt = {e: 0 for e in ENGS}
        self.dcnt = {q: [0] * NDS for q in self.dsem}
        self.dnext = {q: 0 for q in self.dsem}
        self.reset_stage()

    def reset_stage(self):
        self.ops = {e: [] for e in ENGS}
        self.W = {}
        self.R = {}
        self.PR = {}
        self.XW = {}
        self.pos = {e: 0 for e in ENGS}

    def op(self, eng, fn, reads=(), writes=(), dma=False, wa=()):
        deps = []
        for t in reads:
            deps.extend(self.W.get(t, ()))
        for t in writes:
            deps.extend(self.W.get(t, ()))
            deps.extend(self.R.get(t, ()))
        for t in wa:
            if self.R.get(t):
                self.PR[t] = self.R[t]
                self.R[t] = []
                self.W[t] = []
                self.XW[t] = []
            deps.extend(self.PR.get(t, ()))
            deps.extend(self.XW.get(t, ()))
        o = {'eng': eng, 'fn': fn, 'deps': deps, 'dma': dma, 'signal': dma, 'pos': self.pos[eng]}
        self.pos[eng] += 1
        for d in deps:
            d['signal'] = True
        self.ops[eng].append(o)
        for t in reads:
            self.R.setdefault(t, []).append(o)
        for t in writes:
            self.W[t] = [o]
            self.XW[t] = [o]
            self.R[t] = []
            self.PR[t] = []
        for t in wa:
            self.W.setdefault(t, []).append(o)
        return o

    def pe(self, fn, r=(), w=(), wa=()):
        return self.op('pe', fn, r, w, wa=wa)

    def act(self, fn, r=(), w=(), wa=()):
        return self.op('act', fn, r, w, wa=wa)

    def dve(self, fn, r=(), w=(), wa=()):
        return self.op('dve', fn, r, w, wa=wa)

    def pool(self, fn, r=(), w=(), wa=()):
        return self.op('pool', fn, r, w, wa=wa)

    def dma(self, q, out, in_, r=(), w=(), wa=(), **kw):
        return self.op(q, lambda e: e.dma_start(out=out, in_=in_, **kw), r, w, dma=True, wa=wa)

    def flush(self):
        nc = self.nc
        for e in ENGS:
            cops = [o for o in self.ops[e] if not o['dma']]
            if cops:
                cops[-1]['signal'] = True
        for e in ENGS:
            c = self.cnt[e]
            for o in self.ops[e]:
                if o['dma']:
                    slot = self.dnext[e] % NDS
                    self.dnext[e] += 1
                    self.dcnt[e][slot] += 1
                    o['sig'] = (self.dsem[e][slot], 16 * self.dcnt[e][slot])
                    o['slotwait'] = (self.dsem[e][slot], 16 * (self.dcnt[e][slot] - 1))
                elif o['signal']:
                    c += 1
                    o['sig'] = (self.sem[e], c)
            self.cnt[e] = c
        end_c = dict(self.cnt)
        end_d = {q: list(v) for q, v in self.dcnt.items()}

        def emit(e, eng):
            waited = {}

            def wait(sem, val):
                k = id(sem)
                if val <= 0 or waited.get(k, 0) >= val:
                    return
                waited[k] = val
                eng.wait_ge(sem, val)

            for o in self.ops[e]:
                for d in o['deps']:
                    if d is o:
                        continue
                    if d['eng'] == e and not d['dma']:
                        if e == 'pe' or e == 'sp':
                            continue
                        if o['pos'] - d['pos'] > 6:
                            continue
                    wait(*d['sig'])
                if o['dma']:
                    wait(*o['slotwait'])
                ins = o['fn'](eng)
                if o['signal']:
                    s, v = o['sig']
                    ins.then_inc(s, 16 if o['dma'] else 1)
            for e2 in ENGS:
                if e2 != e and e2 != 'sp':
                    wait(self.sem[e2], end_c[e2])
            for q in self.dsem:
                for i in range(NDS):
                    wait(self.dsem[q][i], 16 * end_d[q][i])

        with nc.Block() as block:
            @block.tensor
            def _(eng):
                emit('pe', eng)

            @block.scalar
            def _(eng):
                emit('act', eng)

            @block.vector
            def _(eng):
                emit('dve', eng)

            @block.gpsimd
            def _(eng):
                emit('pool', eng)

            @block.sync
            def _(eng):
                emit('sp', eng)
        self.reset_stage()


def make_consts():
    j = np.arange(128)[:, None]
    c = np.arange(128)[None, :]
    cb = np.arange(896)[None, :]
    tabs = {
        'ident': (j == c),
        'negtri': -1.0 * (j >= c),
        'negones': -np.ones((128, 128)),
        'ones': np.ones((128, 128)),
        'mgt': (j > c),
        'mle': (j <= c),
        'mlt': (j < c),
        'mask01': (j + 384 < cb),
        'negbig': -30000.0 * (~(j + 384 < cb)),
    }
    offs = {}
    cols = []
    o = 0
    for k, v in tabs.items():
        v = np.asarray(v, dtype=np.float32)
        offs[k] = (o, v.shape[1])
        o += v.shape[1]
        cols.append(v)
    full = np.concatenate(cols, axis=1)
    return full.astype(ml_dtypes.bfloat16), offs


CST_BF, CST_OFF = make_consts()
CST_F32 = np.concatenate([np.eye(128, dtype=np.float32), np.tile(np.arange(32, dtype=np.float32)[None, :], (128, 1)),
                          np.arange(128, dtype=np.float32)[:, None]], axis=1)
CST_I32 = (S + (np.arange(128)[:, None] * 8 + np.arange(8)[None, :]) % 128).astype(np.int32)


class Ctx:
    pass


def build_program(stop_after=None, debug=False, only=None):
    nc = bass.Bass("TRN2", target_bir_lowering=False)
    C = Ctx()
    C.nc = nc
    C.debug = debug
    C.nstg = 0

    def din(name, shape, dt=F32):
        return nc.dram_tensor(name, list(shape), dt, kind="ExternalInput").ap()

    def dscr(name, shape, dt):
        return nc.dram_tensor(name, list(shape), dt, kind="ExternalOutput" if debug else "Internal").ap()

    I = {}
    I['x'] = din('x', [S, D])
    I['mem'] = din('mem', [MEM, D])
    I['cst_bf'] = din('cst_bf', list(CST_BF.shape), BF16)
    I['cst_f32'] = din('cst_f32', [128, 161])
    I['cst_i32'] = din('cst_i32', [128, 8], mybir.dt.int32)
    for nm, shp in [('ln_in_g', [D]), ('ln_in_b', [D]), ('w_in', [D, IN_W]), ('b_branch_gate', [2 * D]),
                    ('conv_w', [4, 3072]), ('conv_b', [3072]), ('dt_bias', [32]), ('a_log', [32]),
                    ('d_skip', [32]), ('ssd_norm_g', [2048]), ('w_sb', [D, D]), ('w_ssd', [2048, D]),
                    ('w_mix_out', [D, D]), ('ln1_g', [D]), ('ln1_b', [D]), ('w_xq', [D, D]), ('w_xk', [D, D]),
                    ('w_xv', [D, D]), ('w_xo', [D, D]), ('ln2_g', [D]), ('ln2_b', [D]), ('w_router', [D, NE]),
                    ('b_router', [NE]), ('w_e_gate', [NE, D, D]), ('b_e_gate', [NE * D]), ('w_e_up', [NE, D, D]),
                    ('b_e_up', [NE * D]), ('w_e_down', [NE, D, D]), ('b_e_down', [NE, D]), ('ln3_g', [D]),
                    ('ln3_b', [D])]:
        I[nm] = din(nm, shp)
    C.I = I
    out = nc.dram_tensor('out', [S, D], F32, kind="ExternalOutput").ap()
    C.out = out
    Dm = {}
    Dm['h0'] = dscr('h0_d', [S, D], F32)
    Dm['qT'] = dscr('qT_d', [D, S], BF16)
    Dm['kT'] = dscr('kT_d', [D, S], BF16)
    Dm['v'] = dscr('v_d', [S, D], BF16)
    Dm['z'] = dscr('z_d', [S, 2048], BF16)
    Dm['xs'] = dscr('xs_d', [S, 2048], BF16)
    Dm['btm'] = dscr('btm_d', [S, 512], BF16)
    Dm['bT'] = dscr('bT_d', [512, S], BF16)
    Dm['cT'] = dscr('cT_d', [512, S], BF16)
    Dm['gT'] = dscr('gT_d', [2048, S], BF16)
    Dm['osbT'] = dscr('osbT_d', [D, S], BF16)
    Dm['ossd'] = dscr('ossd_d', [S, 2048], BF16)
    Dm['h1'] = dscr('h1_d', [S, D], F32)
    Dm['h2'] = dscr('h2_d', [S, D], F32)
    C.xrows_d = nc.dram_tensor('xrows_d', [NE * 1024 + 128, 1032], BF16, kind='Internal').ap()
    C.yrows_d = nc.dram_tensor('yrows_d', [NE * 1024 + 128, D], F32, kind='Internal').ap()
    C.ff_d = nc.dram_tensor('ff_d', [S + 128, D], F32, kind='Internal').ap()
    C.D = Dm

    with ExitStack() as top:
        P = Prog(nc, top)
        C.P = P

        def sbt(st, name, shape, dt):
            return st.enter_context(nc.sbuf_tensor(name, list(shape), dt))

        def pst(st, name, shape, dt):
            return st.enter_context(nc.psum_tensor(name, list(shape), dt))

        C.sbt = sbt
        C.pst = pst
        cst = sbt(top, 'cst', list(CST_BF.shape), BF16)
        idf = sbt(top, 'idf', [128, 161], F32)
        C.cst = cst
        C.idf = idf

        def cs(name, lo=0, n=None):
            o, w = CST_OFF[name]
            if n is None:
                n = w
            return cst[:, o + lo:o + lo + n]

        C.cs = cs
        C.ident = cs('ident')
        C.dt_sb = sbt(top, 'dt_sb', [128, NT, 32], F32)
        C.da_bf = sbt(top, 'da_bf', [128, NT, 32], BF16)
        P.dma('sp', cst[:], I['cst_bf'], w=['cst'])
        P.dma('sp', idf[:], I['cst_f32'], w=['idf'])
        P.flush()

        C.ngc, C.bgc, C.buc = load_cols_multi(C, top, [(I['ssd_norm_g'], 16, 'e_ngc'), (I['b_e_gate'], NE * 8, 'g_bg'),
                                                       (I['b_e_up'], NE * 8, 'g_bu')])
        P.dve(lambda e: e.tensor_scalar(C.buc[:], C.buc[:], 1.0, None, ALU.add), w=['g_bu'])
        stages = [stage_ab, stage_attn, stage_ssd, stage_merge, stage_xattn, stage_moe]
        names = ['ab', 'attn', 'ssd', 'merge', 'xattn', 'moe']
        with ExitStack() as hts:
            for fn, nm in zip(stages, names):
                if only is not None and nm not in only:
                    continue
                fn(C)
                if stop_after == nm:
                    break
        if stop_after is not None:
            with ExitStack() as st:
                z = sbt(st, 'zout', [128, D], F32)
                P.dve(lambda e: e.memset(z[:], 0.0), w=['zout'])
                P.dma('sp', out[0:128, :], z[:], r=['zout'])
                P.flush()
    return nc


def ln_stats(C, src, src_tok, stt, junk, key):
    P = C.P
    s = key
    P.dve(lambda e: e.reduce_sum(stt[:, 0:1], src, axis=AX.X), r=[src_tok], w=[s + 's1'])
    P.pool(lambda e: e.memset(stt[:, 1:2], 0.0), w=[s + 's2'])
    P.act(lambda e: e.activation(junk, src, AF.Square, accum_out=stt[:, 1:2]), r=[src_tok], w=[s + 's2', s + 'junk'])
    P.dve(lambda e: e.tensor_scalar(stt[:, 2:3], stt[:, 0:1], 1.0 / D, None, ALU.mult), r=[s + 's1'], w=[s + 'mean'])
    P.dve(lambda e: e.tensor_tensor(stt[:, 3:4], stt[:, 2:3], stt[:, 2:3], ALU.mult), r=[s + 'mean'], w=[s + 'msq'])
    P.dve(lambda e: e.scalar_tensor_tensor(stt[:, 4:5], stt[:, 1:2], 1.0 / D, stt[:, 3:4], ALU.mult, ALU.subtract),
          r=[s + 's2', s + 'msq'], w=[s + 'var'])
    P.dve(lambda e: e.tensor_scalar(stt[:, 4:5], stt[:, 4:5], 0.0, EPS, ALU.max, ALU.add), r=[s + 'var'], w=[s + 'var'])
    P.act(lambda e: e.activation(stt[:, 5:6], stt[:, 4:5], AF.Ln), r=[s + 'var'], w=[s + 'lnv'])
    P.act(lambda e: e.activation(stt[:, 6:7], stt[:, 5:6], AF.Exp, scale=-0.5), r=[s + 'lnv'], w=[s + 'rstd'])
    P.dve(lambda e: e.scalar_tensor_tensor(stt[:, 7:8], stt[:, 2:3], -1.0, stt[:, 6:7], ALU.mult, ALU.mult),
          r=[s + 'mean', s + 'rstd'], w=[s + 'nmr'])


def ln_apply(C, src, src_tok, dst, dst_tok, g_bc, b_bc, stt, key):
    P = C.P
    s = key
    P.act(lambda e: e.activation(dst, src, AF.Identity, scale=stt[:, 6:7], bias=stt[:, 7:8]),
          r=[src_tok, s + 'rstd', s + 'nmr'], w=[dst_tok])
    P.dve(lambda e: e.tensor_tensor(dst, dst, g_bc, ALU.mult), r=[dst_tok, 'lnp'], w=[dst_tok])
    P.dve(lambda e: e.tensor_tensor(dst, dst, b_bc, ALU.add), r=[dst_tok, 'lnp'], w=[dst_tok])


def ln_tile(C, src, src_tok, dst, dst_tok, g_bc, b_bc, stt, junk, key):
    ln_stats(C, src, src_tok, stt, junk, key)
    ln_apply(C, src, src_tok, dst, dst_tok, g_bc, b_bc, stt, key)


def load_ln_params(C, g_d, b_d, g_bc, b_bc):
    C.P.dma('sp', g_bc[:], g_d.partition_broadcast(128), wa=['lnp'])
    C.P.dma('sp', b_bc[:], b_d.partition_broadcast(128), wa=['lnp'])


def load_cols_multi(C, st, specs):
    P = C.P
    outs = [C.sbt(st, name, [128, ncols], F32) for (_, ncols, name) in specs]
    with ExitStack() as tmp:
        tag = specs[0][2]
        ps = C.pst(tmp, 'lc_ps_' + tag, [128, 512], F32)
        nr = 0
        rows = [C.sbt(tmp, 'lc_rows_%s%d' % (tag, i), [128, 128], F32) for i in range(4)]
        for (vec_d, ncols, name), outt in zip(specs, outs):
            v2 = vec_d.rearrange("(c p) -> c p", p=128)
            done = 0
            while done < ncols:
                n = min(128, ncols - done)
                rb = nr % 4
                nr += 1
                tk = 'lc_r%d' % rb
                P.dma('sp', rows[rb][0:n, :], v2[done:done + n, :], w=[tk])
                P.pe(lambda e, rb=rb, n=n: e.transpose(ps[:, 0:n], rows[rb][0:n, :], C.idf[0:n, 0:n]),
                     r=[tk, 'idf'], w=['lc_ps'])
                P.dve(lambda e, n=n, d0=done, outt=outt: e.tensor_copy(outt[:, d0:d0 + n], ps[:, 0:n]),
                      w=['lc_ps'], wa=[name])
                done += n
        P.flush()
    return outs


def stage_ab(C):
    nc, P, I, Dm = C.nc, C.P, C.I, C.D
    with ExitStack() as st:
        hT = C.sbt(st, 'hT_ab', [128, 8, S], BF16)
        lc = load_cols_multi(C, st, [(I['b_branch_gate'], 16, 'bgate')] + [(I['conv_w'][k], 24, 'convw%d' % k) for k in range(4)]
                             + [(I['conv_b'], 24, 'convb')])
        bgate, convw, convb = lc[0], lc[1:5], lc[5]
        with ExitStack() as sa:
            g_bc = C.sbt(sa, 'a_g', [128, D], F32)
            b_bc = C.sbt(sa, 'a_b', [128, D], F32)
            load_ln_params(C, I['ln_in_g'], I['ln_in_b'], g_bc, b_bc)
            xin = [C.sbt(sa, 'a_x%d' % i, [128, D], F32) for i in range(2)]
            hh = [C.sbt(sa, 'a_h%d' % i, [128, D], F32) for i in range(2)]
            hb = [C.sbt(sa, 'a_hb%d' % i, [128, D], BF16) for i in range(2)]
            stt = [C.sbt(sa, 'a_st%d' % i, [128, 8], F32) for i in range(2)]
            junk = C.sbt(sa, 'a_junk', [128, D], BF16)
            ptr = [C.pst(sa, 'a_ptr%d' % i, [128, 8, 128], BF16) for i in range(2)]
            def a_p1(i):
                b = i % 2
                P.dma('sp', xin[b][:], I['x'][i * 128:(i + 1) * 128, :], w=['xin%d' % b])
                ln_stats(C, xin[b][:], 'xin%d' % b, stt[b], junk[:], 'a%d' % b)

            a_p1(0)
            for i in range(NT):
                b = i % 2
                if i + 1 < NT:
                    a_p1(i + 1)
                ln_apply(C, xin[b][:], 'xin%d' % b, hh[b][:], 'hh%d' % b, g_bc[:], b_bc[:], stt[b], 'a%d' % b)
                P.dma('pool', Dm['h0'][i * 128:(i + 1) * 128, :], hh[b][:], r=['hh%d' % b])
                P.act(lambda e, b=b: e.copy(hb[b][:], hh[b][:]), r=['hh%d' % b], w=['hb%d' % b])
                for c in range(8):
                    P.pe(lambda e, b=b, c=c: e.transpose(ptr[b][:, c, :], hb[b][:, c * 128:(c + 1) * 128], C.ident),
                         r=['hb%d' % b], w=['ptr%d' % b])
                P.dve(lambda e, b=b, i=i: e.tensor_copy(hT[:, :, i * 128:(i + 1) * 128], ptr[b][:]),
                      w=['ptr%d' % b, 'hT'])
            P.flush()
        with ExitStack() as sb:
            wblk = [C.sbt(sb, 'b_w%d' % i, [128, 8, 512], BF16) for i in range(2)]
            ps = [C.pst(sb, 'b_ps%d' % i, [128, 512], F32) for i in range(4)]
            stg = [C.sbt(sb, 'b_stg%d' % i, [128, S], BF16) for i in range(2)]
            nblk = [0]
            npsum = [0]
            nstg = [0]

            wlist = ([(c, 512) for c in (0, 512, 1024, 1536)] + [(8224 + i * 512, 512) for i in range(4)]
                     + [(2048, 512), (2560, 512)] + [(3072 + i * 512, 512) for i in range(4)] + [(8192, 32)]
                     + [(5120 + i * 512, 512) for i in range(6)])
            wissued = [0]

            def issue_w(n):
                c0, ncols = wlist[n]
                b = n % 2
                P.dma('pool', wblk[b][:, :, 0:ncols],
                      I['w_in'][:, c0:c0 + ncols].rearrange("(k p) c -> p k c", p=128), w=['wblk%d' % b])

            def load_w(c0, ncols):
                n = nblk[0]
                nblk[0] += 1
                assert wlist[n] == (c0, ncols), (n, wlist[n], c0, ncols)
                while wissued[0] <= min(n + 1, len(wlist) - 1):
                    issue_w(wissued[0])
                    wissued[0] += 1
                return n % 2

            def mm_fm(wb, cc, tg):
                pb = npsum[0] % 4
                npsum[0] += 1
                for k in range(8):
                    P.pe(lambda e, k=k, pb=pb: e.matmul(ps[pb][:], wblk[wb][:, k, cc * 128:(cc + 1) * 128],
                                                        hT[:, k, tg * 512:(tg + 1) * 512], start=(k == 0), stop=(k == 7)),
                         r=['wblk%d' % wb, 'hT'], w=['bps%d' % pb])
                return pb

            def fm_group(c0, ncols_total, dst, evac):
                for blk in range(ncols_total // 512):
                    wb = load_w(c0 + blk * 512, 512)
                    for cc in range(4):
                        sgb = nstg[0] % 2
                        nstg[0] += 1
                        chunk = blk * 4 + cc
                        for tg in range(8):
                            pb = mm_fm(wb, cc, tg)
                            evac(pb, stg[sgb][:, tg * 512:(tg + 1) * 512], chunk, 'stg%d' % sgb, tg)
                        P.dma('sp', dst[chunk * 128:(chunk + 1) * 128, :], stg[sgb][:], r=['stg%d' % sgb])

            def evac_copy(pb, dst, chunk, tok, tg):
                if tg % 2 == 0:
                    P.act(lambda e: e.copy(dst, ps[pb][:]), w=['bps%d' % pb], wa=[tok])
                else:
                    P.dve(lambda e: e.tensor_copy(dst, ps[pb][:]), w=['bps%d' % pb], wa=[tok])

            def evac_gate(pb, dst, chunk, tok, tg):
                P.act(lambda e: e.activation(dst, ps[pb][:], AF.Sigmoid, bias=bgate[:, chunk:chunk + 1]),
                      r=['bgate'], w=['bps%d' % pb], wa=[tok])

            fm_group(0, 1024, Dm['qT'], evac_copy)
            fm_group(1024, 1024, Dm['kT'], evac_copy)
            fm_group(8224, 2048, Dm['gT'], evac_gate)

            tstg = [C.sbt(sb, 'b_tstg%d' % i, [128, 4, 512], BF16) for i in range(2)]
            ntst = [0]

            def tm_group(c0, ncols_total, dst):
                for blk in range(ncols_total // 512):
                    wb = load_w(c0 + blk * 512, 512)
                    for t4 in range(NT // 4):
                        tb = ntst[0] % 2
                        ntst[0] += 1
                        for tt in range(4):
                            ti = t4 * 4 + tt
                            pb = npsum[0] % 4
                            npsum[0] += 1
                            for k in range(8):
                                P.pe(lambda e, k=k, pb=pb, ti=ti, wb=wb: e.matmul(ps[pb][:], hT[:, k, ti * 128:(ti + 1) * 128],
                                                                                  wblk[wb][:, k, :], start=(k == 0), stop=(k == 7)),
                                     r=['wblk%d' % wb, 'hT'], w=['bps%d' % pb])
                            if tt % 2 == 0:
                                P.act(lambda e, pb=pb, tb=tb, tt=tt: e.copy(tstg[tb][:, tt, :], ps[pb][:]),
                                      w=['bps%d' % pb], wa=['tstg%d' % tb])
                            else:
                                P.dve(lambda e, pb=pb, tb=tb, tt=tt: e.tensor_copy(tstg[tb][:, tt, :], ps[pb][:]),
                                      w=['bps%d' % pb], wa=['tstg%d' % tb])
                        P.dma('sp', dst[t4 * 512:(t4 + 1) * 512, blk * 512:(blk + 1) * 512].rearrange("(t p) c -> p t c", p=128),
                              tstg[tb][:], r=['tstg%d' % tb])

            tm_group(2048, 1024, Dm['v'])
            tm_group(3072, 2048, Dm['z'])

            dtb = C.sbt(sb, 'b_dtb', [128, 32], F32)
            abc = C.sbt(sb, 'b_abc', [128, 32], F32)
            dtt = [C.sbt(sb, 'b_dtt%d' % i, [128, 32], F32) for i in range(2)]
            P.dma('sp', dtb[:], I['dt_bias'].partition_broadcast(128), w=['dtb'])
            P.dma('sp', abc[:], I['a_log'].partition_broadcast(128), w=['abc'])
            P.act(lambda e: e.activation(abc[:], abc[:], AF.Exp), r=['abc'], w=['abc'])
            P.dve(lambda e: e.tensor_scalar(abc[:], abc[:], -1.0, None, ALU.mult), r=['abc'], w=['abc'])
            wb = load_w(8192, 32)
            for ti in range(NT):
                pb = npsum[0] % 4
                npsum[0] += 1
                b = ti % 2
                for k in range(8):
                    P.pe(lambda e, k=k, pb=pb, ti=ti, wb=wb: e.matmul(ps[pb][:, 0:32], hT[:, k, ti * 128:(ti + 1) * 128],
                                                                      wblk[wb][:, k, 0:32], start=(k == 0), stop=(k == 7)),
                         r=['wblk%d' % wb, 'hT'], w=['bps%d' % pb])
                P.dve(lambda e, pb=pb, b=b: e.tensor_tensor(dtt[b][:], ps[pb][:, 0:32], dtb[:], ALU.add),
                      r=['dtb'], w=['bps%d' % pb, 'dtt%d' % b])
                P.act(lambda e, b=b: e.activation(dtt[b][:], dtt[b][:], AF.Exp), r=['dtt%d' % b], w=['dtt%d' % b])
                P.act(lambda e, b=b, ti=ti: e.activation(C.dt_sb[:, ti, :], dtt[b][:], AF.Ln, bias=1.0),
                      r=['dtt%d' % b], w=['dt_sb'])
                P.dve(lambda e, ti=ti: e.tensor_tensor(C.da_bf[:, ti, :], C.dt_sb[:, ti, :], abc[:], ALU.mult),
                      r=['dt_sb', 'abc'], w=['da_bf'])

            U = [C.sbt(sb, 'b_U%d' % i, [128, S + 3], BF16) for i in range(2)]
            dg = C.sbt(sb, 'b_dg', [128, 96, 128], BF16)
            tps = [C.pst(sb, 'b_tps%d' % i, [128, 8, 128], BF16) for i in range(2)]
            cvp = [C.pst(sb, 'b_cvp%d' % i, [128, 512], F32) for i in range(2)]
            xtm = [C.sbt(sb, 'b_xtm%d' % i, [128, NT, 128], BF16) for i in range(2)]
            for i in range(2):
                P.dve(lambda e, i=i: e.memset(U[i][:, 0:3], 0.0), w=['U%d_m' % i])
            for chunk in range(24):
                for k in range(4):
                    P.dve(lambda e, chunk=chunk, k=k: e.tensor_scalar(dg[:, chunk * 4 + k, :], C.ident, convw[k][:, chunk:chunk + 1], None, ALU.mult),
                          r=['cst', 'convw%d' % k], wa=['dg'])
            ntp = [0]
            ncv = [0]
            for blk in range(6):
                wb = load_w(5120 + blk * 512, 512)
                for cc in range(4):
                    chunk = blk * 4 + cc
                    ub = chunk % 2

                    def conv_tg(tg, chunk=chunk, ub=ub):
                        cb = ncv[0] % 2
                        ncv[0] += 1
                        rt = ['U%d_%d' % (ub, tg), 'dg'] + (['U%d_%d' % (ub, tg - 1)] if tg > 0 else ['U%d_m' % ub])
                        for k in range(4):
                            P.pe(lambda e, k=k, cb=cb: e.matmul(cvp[cb][:], dg[:, chunk * 4 + k, :], U[ub][:, tg * 512 + k:tg * 512 + k + 512],
                                                                start=(k == 0), stop=(k == 3)), r=rt, w=['cvp%d' % cb])
                        P.act(lambda e, cb=cb: e.activation(stg[ub][:, tg * 512:(tg + 1) * 512], cvp[cb][:], AF.Silu, bias=convb[:, chunk:chunk + 1]),
                              r=['convb'], w=['cvp%d' % cb], wa=['stg%d' % ub])

                    for tg in range(8):
                        pb = mm_fm(wb, cc, tg)
                        if tg % 2 == 0:
                            P.act(lambda e, pb=pb, ub=ub, tg=tg: e.copy(U[ub][:, 3 + tg * 512:3 + (tg + 1) * 512], ps[pb][:]),
                                  w=['bps%d' % pb, 'U%d_%d' % (ub, tg)])
                        else:
                            P.dve(lambda e, pb=pb, ub=ub, tg=tg: e.tensor_copy(U[ub][:, 3 + tg * 512:3 + (tg + 1) * 512], ps[pb][:]),
                                  w=['bps%d' % pb, 'U%d_%d' % (ub, tg)])
                        if tg > 0:
                            conv_tg(tg - 1)
                    conv_tg(7)
                    if 16 <= chunk < 20:
                        P.dma('sp', Dm['bT'][(chunk - 16) * 128:(chunk - 15) * 128, :], stg[ub][:], r=['stg%d' % ub])
                    if chunk >= 20:
                        P.dma('sp', Dm['cT'][(chunk - 20) * 128:(chunk - 19) * 128, :], stg[ub][:], r=['stg%d' % ub])
                    if chunk < 20:
                        for t8 in range(4):
                            tb = ntp[0] % 2
                            ntp[0] += 1
                            for j in range(8):
                                ti = t8 * 8 + j
                                P.pe(lambda e, tb=tb, j=j, ti=ti, ub=ub: e.transpose(
                                    tps[tb][:, j, :], stg[ub][:, ti * 128:(ti + 1) * 128], C.ident),
                                    r=['stg%d' % ub], w=['tps%d' % tb])
                            if t8 % 2 == 0:
                                P.dve(lambda e, tb=tb, t8=t8, ub=ub: e.tensor_copy(xtm[ub][:, t8 * 8:(t8 + 1) * 8, :], tps[tb][:]),
                                      w=['tps%d' % tb], wa=['xtm%d' % ub])
                            else:
                                P.act(lambda e, tb=tb, t8=t8, ub=ub: e.copy(xtm[ub][:, t8 * 8:(t8 + 1) * 8, :], tps[tb][:]),
                                      w=['tps%d' % tb], wa=['xtm%d' % ub])
                        if chunk < 16:
                            dst = Dm['xs'][:, chunk * 128:(chunk + 1) * 128]
                        else:
                            dst = Dm['btm'][:, (chunk - 16) * 128:(chunk - 15) * 128]
                        P.dma('sp', dst.rearrange("(t p) c -> p t c", p=128), xtm[ub][:], r=['xtm%d' % ub])
            P.flush()


def stage_attn(C):
    nc, P, Dm = C.nc, C.P, C.D
    cs = C.cs
    with ExitStack() as st:
        V = C.sbt(st, 'c_V', [128, NT, D], BF16)
        QT = [C.sbt(st, 'c_QT%d' % i, [64, S], BF16) for i in range(2)]
        KT = [C.sbt(st, 'c_KT%d' % i, [64, S], BF16) for i in range(2)]
        osb = [C.sbt(st, 'c_osb%d' % i, [64, S], BF16) for i in range(2)]
        NB = 5
        E = [C.sbt(st, 'c_E%d' % i, [128, 512], F32) for i in range(NB)]
        SP = [C.sbt(st, 'c_SP%d' % i, [128, 512], BF16) for i in range(NB)]
        WT = [C.sbt(st, 'c_WT%d' % i, [128, 512], BF16) for i in range(NB)]
        LA = [C.sbt(st, 'c_LA%d' % i, [128, 512], BF16) for i in range(2)]
        zps = [C.pst(st, 'c_zps%d' % i, [128, 512], F32) for i in range(3)]
        cps = [C.pst(st, 'c_cps%d' % i, [128, 512], F32) for i in range(3)]
        ops_ = [C.pst(st, 'c_ops%d' % i, [128, 512], F32) for i in range(2)]
        for i in range(NB):
            P.dve(lambda e, i=i: e.memset(SP[i][:], 0.0), w=['SP%d' % i])
        vv = Dm['v'].rearrange("(t p) c -> p t c", p=128)
        I = C.I
        zt = C.sbt(st, 'zt', [128, 8 * 1032], BF16)
        P.pool(lambda e: e.memset(zt[:], 0.0), w=['zt'])
        zid = C.sbt(st, 'zid', [128, 8], mybir.dt.int32)
        P.dma('sp', zid[:], I['cst_i32'], w=['zid'])
        for j in range(8):
            P.pool(lambda e, j=j: e.tensor_copy(zt[:, j * 1032 + 1026:j * 1032 + 1028].bitcast(mybir.dt.int32), zid[:, j:j + 1]),
                   r=['zid'], w=['zt'])
        ztf = zt[:].bitcast(F32)
        zfills = [(C.xrows_d[i * 1024:(i + 1) * 1024, :].rearrange("(p j) c -> p (j c)", p=128), zt[:]) for i in range(32)]
        zfills += [(C.ff_d[i * 512:(i + 1) * 512, :].rearrange("(p j) c -> p (j c)", p=128), ztf[:, 0:4096]) for i in range(8)]
        tiles = []
        nqr = 0
        for h in range(16):
            for qr in range(8):
                nb = 4 * (qr + 1)
                for bi, kb in enumerate(range(nb - 1, -1, -1)):
                    tiles.append(dict(h=h, hb=h % 2, qr=qr, q0=qr * 512, bi=bi, kb=kb, nb=nb, ob=nqr % 2,
                                      diag=(kb >= 4 * qr), off=kb * 128 - qr * 512, lo=max(0, kb * 128 - qr * 512), last=(bi == nb - 1), i=len(tiles)))
                nqr += 1

        def load_head(h):
            hb = h % 2
            P.dma('sp', QT[hb][:], Dm['qT'][h * 64:(h + 1) * 64, :], w=['QT%d' % hb])
            P.dma('sp', KT[hb][:], Dm['kT'][h * 64:(h + 1) * 64, :], w=['KT%d' % hb])
            P.dve(lambda e, hb=hb: e.tensor_scalar(KT[hb][:], KT[hb][:], 0.125, None, ALU.mult),
                  r=['KT%d' % hb], w=['KT%d' % hb])

        def stage_a(t):
            i = t['i']
            zb = i % 3
            b4 = i % NB
            hb = t['hb']
            if t['h'] == 0 and t['qr'] == 0 and t['bi'] == 0:
                load_head(0)
                for q4 in range(4):
                    P.dma('sp', V[:, q4 * 8:(q4 + 1) * 8, :], vv[:, q4 * 8:(q4 + 1) * 8, :], wa=['V'])
            if t['qr'] == 1 and t['bi'] == 0 and t['h'] + 1 < 16:
                load_head(t['h'] + 1)
            lo = t['lo']
            ks = KT[hb][:, t['kb'] * 128:(t['kb'] + 1) * 128]
            qs = QT[hb][:, t['q0'] + lo:t['q0'] + 512]
            P.pe(lambda e: e.matmul(zps[zb][:, lo:512], ks, qs, start=True, stop=True), r=['QT%d' % hb, 'KT%d' % hb], w=['zps%d' % zb])
            P.act(lambda e: e.activation(E[b4][:, lo:512], zps[zb][:, lo:512], AF.Exp), w=['zps%d' % zb, 'E%d' % b4])

        def stage_a2(t):
            i = t['i']
            b4 = i % NB
            lo = t['lo']
            P.act(lambda e: e.activation(SP[b4][:, lo:512], E[b4][:, lo:512], AF.Ln, bias=1.0), r=['E%d' % b4], w=['SP%d' % b4])
            if t['diag']:
                off = t['off']
                if lo > 0:
                    P.dve(lambda e: e.memset(WT[b4][:, 0:lo], 0.0), w=['WT%d' % b4])
                P.dve(lambda e: e.tensor_tensor(SP[b4][:], SP[b4][:], cs('mask01', 384 - off, 512), ALU.mult),
                      r=['SP%d' % b4, 'cst'], w=['SP%d' % b4])

        def stage_b(t):
            i = t['i']
            cb = i % 3
            b4 = i % NB
            hb = t['hb']
            ob = t['ob']
            lo = t['lo']
            ks = KT[hb][:, t['kb'] * 128:(t['kb'] + 1) * 128]
            qs = QT[hb][:, t['q0'] + lo:t['q0'] + 512]
            mms = [(ks, qs, ['QT%d' % hb, 'KT%d' % hb]), (cs('negtri'), SP[b4][:, lo:512], ['cst', 'SP%d' % b4])]
            if t['bi'] > 0:
                mms.append((cs('negones'), LA[ob][:, lo:512], ['cst', 'LA%d' % ob]))
            if t['diag']:
                mms.append((C.ident, cs('negbig', 384 - t['off'] + lo, 512 - lo), ['cst']))
            for j, (l, r_, tk) in enumerate(mms):
                P.pe(lambda e, l=l, r_=r_, j=j, n=len(mms): e.matmul(cps[cb][:, lo:512], l, r_, start=(j == 0), stop=(j == n - 1)),
                     r=tk, w=['cps%d' % cb])
            P.act(lambda e: e.activation(WT[b4][:, lo:512], cps[cb][:, lo:512], AF.Exp), w=['cps%d' % cb], wa=['WT%d' % b4])
            if not t['last']:
                if t['bi'] == 0:
                    P.dve(lambda e: e.tensor_copy(LA[ob][:], SP[b4][:]), r=['SP%d' % b4], w=['LA%d' % ob])
                else:
                    P.dve(lambda e: e.tensor_tensor(LA[ob][:], LA[ob][:], SP[b4][:], ALU.add),
                           r=['SP%d' % b4, 'LA%d' % ob], w=['LA%d' % ob])

        def stage_c(t):
            i = t['i']
            b4 = i % NB
            hb = t['hb']
            ob = t['ob']
            h = t['h']
            kb = t['kb']
            q0 = t['q0']
            P.pe(lambda e: e.matmul(ops_[ob][0:64, :], V[:, kb, h * 64:(h + 1) * 64], WT[b4][:], start=(t['bi'] == 0), stop=t['last']),
                 r=['V', 'WT%d' % b4], w=['ops%d' % ob])
            if t['last']:
                P.dve(lambda e: e.tensor_copy(osb[hb][:, q0:q0 + 512], ops_[ob][0:64, :]), w=['ops%d' % ob, 'osb%d' % hb])
                if t['qr'] == 7:
                    P.dma('sp', Dm['osbT'][h * 64:(h + 1) * 64, :], osb[hb][:], r=['osb%d' % hb])

        n = len(tiles)
        for s_ in range(n + 3):
            if s_ == 4:
                for zo, zi in zfills:
                    P.dma('sp', zo, zi, r=['zt'])
            if s_ < n:
                stage_a(tiles[s_])
            if 0 <= s_ - 1 < n:
                stage_a2(tiles[s_ - 1])
            if 0 <= s_ - 2 < n:
                stage_b(tiles[s_ - 2])
            if 0 <= s_ - 3 < n:
                stage_c(tiles[s_ - 3])
        P.flush()


def stage_ssd(C):
    nc, P, I, Dm = C.nc, C.P, C.I, C.D
    cs = C.cs
    with ExitStack() as st:
        sbt = lambda n, s, d: C.sbt(st, n, s, d)
        pst = lambda n, s, d: C.pst(st, n, s, d)
        hst = sbt('d_hst', [128, 2048], F32)
        hbf = sbt('d_hbf', [128, 2048], BF16)
        dsk = sbt('d_dsk', [128, 32], F32)
        mtge = sbt('d_mtge', [128, 128], F32)
        xs = [sbt('d_xs%d' % i, [128, 2048], BF16) for i in range(2)]
        zz = [sbt('d_zz%d' % i, [128, 2048], BF16) for i in range(2)]
        btm = [sbt('d_btm%d' % i, [128, 512], BF16) for i in range(2)]
        bT = [sbt('d_bT%d' % i, [128, 4, 128], BF16) for i in range(2)]
        cT = [sbt('d_cT%d' % i, [128, 4, 128], BF16) for i in range(2)]
        ex3 = [sbt('d_ex3%d' % i, [128, 96], F32) for i in range(2)]
        xdt = [sbt('d_xdt%d' % i, [128, 2048], BF16) for i in range(2)]
        xw = [sbt('d_xw%d' % i, [128, 2048], BF16) for i in range(2)]
        rbig = [sbt('d_rbig%d' % i, [128, 4096], BF16) for i in range(2)]
        seg = [sbt('d_seg%d' % i, [128, 1024], BF16) for i in range(2)]
        GT4 = [sbt('d_GT%d' % i, [128, 4, 1024], BF16) for i in range(2)]
        cbm = [sbt('d_cbm%d' % i, [128, 128], BF16) for i in range(2)]
        t1 = [sbt('d_t1%d' % i, [128, 512], F32) for i in range(2)]
        t2 = [sbt('d_t2%d' % i, [128, 512], F32) for i in range(2)]
        ysb = [sbt('d_y%d' % i, [128, 2048], F32) for i in range(2)]
        dskI = sbt('d_dskI', [128, 32, 128], BF16)
        sz = [sbt('d_sz%d' % i, [128, 2048], F32) for i in range(2)]
        sst = sbt('d_sst', [128, 8], F32)
        jnk = sbt('d_jnk', [128, 512], BF16)
        ob = [sbt('d_ob%d' % i, [128, 2048], BF16) for i in range(2)]
        sm_ps = pst('d_smps', [128, 512], F32)
        S_ps = pst('d_Sps', [128, 1024], F32)
        cb_ps = pst('d_cbps', [128, 512], F32)
        y_ps = pst('d_yps', [128, 512], F32)
        yi_ps = pst('d_yips', [128, 512], F32)
        hn_ps = pst('d_hnps', [128, 512], F32)

        P.dma('sp', dsk[:], I['d_skip'].partition_broadcast(128), w=['dsk'])
        P.dve(lambda e: e.memset(hst[:], 0.0), w=['hst%d' % g for g in range(4)])
        P.dve(lambda e: e.memset(hbf[:], 0.0), w=['hbf%d' % g for g in range(4)])
        P.dve(lambda e: e.tensor_copy(mtge[:], cs('mle')), r=['cst'], w=['mtge'])
        for h in range(32):
            P.dve(lambda e, h=h: e.tensor_scalar(dskI[:, h, :], C.ident, dsk[:, h:h + 1], None, ALU.mult), r=['cst', 'dsk'], wa=['dskI'])
        bTv = Dm['bT'].rearrange("(g n) t -> n g t", n=128)
        cTv = Dm['cT'].rearrange("(g n) t -> n g t", n=128)
        def stage1(c):
            b = c % 2
            r0 = c * 128
            P.dma('sp', xs[b][:], Dm['xs'][r0:r0 + 128, :], w=['xs%d' % b])
            P.dma('sp', zz[b][:], Dm['z'][r0:r0 + 128, :], w=['zz%d' % b])
            P.dma('sp', btm[b][:], Dm['btm'][r0:r0 + 128, :], w=['btm%d' % b])
            P.dma('sp', bT[b][:], bTv[:, :, r0:r0 + 128], w=['bT%d' % b])
            P.dma('sp', cT[b][:], cTv[:, :, r0:r0 + 128], w=['cT%d' % b])
            da_c = C.da_bf[:, c, :]
            dt_c = C.dt_sb[:, c, :]
            P.pe(lambda e: e.matmul(sm_ps[:, 0:32], cs('mle'), da_c, start=True, stop=True), r=['cst'], w=['smps'])
            P.pe(lambda e: e.matmul(sm_ps[:, 32:64], cs('mgt'), da_c, start=True, stop=True), r=['cst'], w=['smps'])
            P.pe(lambda e: e.matmul(sm_ps[:, 64:96], cs('ones'), da_c, start=True, stop=True), r=['cst'], w=['smps'])
            P.act(lambda e: e.activation(ex3[b][:], sm_ps[:, 0:96], AF.Exp), w=['smps', 'ex3%d' % b])
            P.dve(lambda e: e.tensor_tensor(
                xdt[b][:].rearrange("p (h c) -> p h c", h=32), xs[b][:].rearrange("p (h c) -> p h c", h=32),
                dt_c.unsqueeze(2).to_broadcast([128, 32, 64]), ALU.mult), r=['xs%d' % b], w=['xdt%d' % b])
            P.pool(lambda e: e.tensor_tensor(
                xw[b][:].rearrange("p (h c) -> p h c", h=32), xdt[b][:].rearrange("p (h c) -> p h c", h=32),
                ex3[b][:, 32:64].unsqueeze(2).to_broadcast([128, 32, 64]), ALU.mult),
                r=['xdt%d' % b, 'ex3%d' % b], w=['xw%d' % b])
            P.dve(lambda e: e.tensor_tensor(
                rbig[b][:].rearrange("p (h t) -> p h t", h=32), cs('mle').unsqueeze(1).to_broadcast([128, 32, 128]),
                da_c.unsqueeze(2).to_broadcast([128, 32, 128]), ALU.mult), r=['cst'], w=['rbig%d' % b])
            P.act(lambda e: e.activation(sz[b][:], zz[b][:], AF.Silu), r=['zz%d' % b], w=['sz%d' % b])
            for g in range(4):
                gb = (c * 4 + g) % 2
                for hf in range(2):
                    P.pe(lambda e, g=g, hf=hf: e.matmul(S_ps[:, hf * 512:(hf + 1) * 512], cs('mgt'),
                                                        rbig[b][:, g * 1024 + hf * 512:g * 1024 + (hf + 1) * 512],
                                                        start=True, stop=True), r=['cst', 'rbig%d' % b], w=['Sps'])
                P.act(lambda e, gb=gb: e.activation(seg[gb][:], S_ps[:], AF.Exp), w=['Sps', 'seg%d' % gb])
                P.pe(lambda e, g=g: e.matmul(cb_ps[:, 0:128], bT[b][:, g, :], cT[b][:, g, :], start=True, stop=True),
                     r=['bT%d' % b, 'cT%d' % b], w=['cbps'])
                P.dve(lambda e, gb=gb: e.tensor_tensor(cbm[gb][:], cb_ps[:, 0:128], mtge[:], ALU.mult), r=['mtge'], w=['cbps', 'cbm%d' % gb])
                eng = P.pool
                eng(lambda e, gb=gb, g=g: e.tensor_tensor(
                    GT4[b][:, g, :].rearrange("p (r t) -> p r t", r=8), seg[gb][:].rearrange("p (r t) -> p r t", r=8),
                    cbm[gb][:].unsqueeze(1).to_broadcast([128, 8, 128]), ALU.mult),
                    r=['seg%d' % gb, 'cbm%d' % gb], w=['GT%d%d' % (b, g)])

        def stage2(c):
            b = c % 2
            r0 = c * 128
            for g in range(4):
                gb = (c * 4 + g) % 2
                for r in range(8):
                    hh = g * 8 + r
                    P.pe(lambda e, g=g, r=r, hh=hh: e.matmul(
                        y_ps[:, r * 64:(r + 1) * 64], GT4[b][:, g, r * 128:(r + 1) * 128], xdt[b][:, hh * 64:(hh + 1) * 64],
                        start=True, stop=False), r=['GT%d%d' % (b, g), 'xdt%d' % b], w=['yps'])
                    P.pe(lambda e, r=r, hh=hh: e.matmul(
                        y_ps[:, r * 64:(r + 1) * 64], dskI[:, hh, :], xs[b][:, hh * 64:(hh + 1) * 64],
                        start=False, stop=True), r=['dskI', 'xs%d' % b], w=['yps'])
                P.pe(lambda e, g=g: e.matmul(yi_ps[:], cT[b][:, g, :], hbf[:, g * 512:(g + 1) * 512], start=True, stop=True),
                     r=['cT%d' % b, 'hbf%d' % g], w=['yips'])
                P.dve(lambda e, gb=gb, g=g: e.tensor_tensor(
                    t1[gb][:].rearrange("p (r c) -> p r c", r=8), yi_ps[:].rearrange("p (r c) -> p r c", r=8),
                    ex3[b][:, g * 8:(g + 1) * 8].unsqueeze(2).to_broadcast([128, 8, 64]), ALU.mult),
                    r=['ex3%d' % b], w=['yips', 't1%d' % gb])
                P.dve(lambda e, gb=gb, g=g: e.tensor_tensor(ysb[b][:, g * 512:(g + 1) * 512], t1[gb][:], y_ps[:], ALU.add),
                      r=['t1%d' % gb], w=['yps', 'ysb%d%d' % (b, g)])
                P.pe(lambda e, g=g: e.matmul(hn_ps[:], btm[b][:, g * 128:(g + 1) * 128], xw[b][:, g * 512:(g + 1) * 512],
                                             start=True, stop=True), r=['btm%d' % b, 'xw%d' % b], w=['hnps'])
                P.dve(lambda e, gb=gb, g=g: e.tensor_tensor(
                    t2[gb][:].rearrange("p (r c) -> p r c", r=8), hst[:, g * 512:(g + 1) * 512].rearrange("p (r c) -> p r c", r=8),
                    ex3[b][:, 64 + g * 8:64 + (g + 1) * 8].unsqueeze(2).to_broadcast([128, 8, 64]), ALU.mult),
                    r=['hst%d' % g, 'ex3%d' % b], w=['t2%d' % gb])
                P.dve(lambda e, gb=gb, g=g: e.tensor_tensor(hst[:, g * 512:(g + 1) * 512], t2[gb][:], hn_ps[:], ALU.add),
                      r=['t2%d' % gb], w=['hnps', 'hst%d' % g])
                P.act(lambda e, g=g: e.copy(hbf[:, g * 512:(g + 1) * 512], hst[:, g * 512:(g + 1) * 512]),
                      r=['hst%d' % g], w=['hbf%d' % g])
            ytoks = ['ysb%d%d' % (b, g) for g in range(4)]
            P.dve(lambda e: e.tensor_tensor(ysb[b][:], ysb[b][:], sz[b][:], ALU.mult), r=ytoks + ['sz%d' % b], w=ytoks)
            P.pool(lambda e: e.memset(sst[:, 0:4], 0.0), w=['ss%d' % g for g in range(4)])
            for g in range(4):
                P.act(lambda e, g=g: e.activation(jnk[:], ysb[b][:, g * 512:(g + 1) * 512], AF.Square,
                                                  accum_out=sst[:, g:g + 1]), r=ytoks, w=['jnk', 'ss%d' % g])
            sstk = ['ss%d' % g for g in range(4)]
            P.dve(lambda e: e.tensor_scalar(sst[:, 0:4], sst[:, 0:4], 1.0 / 512, EPS, ALU.mult, ALU.add), r=sstk, w=sstk)
            P.act(lambda e: e.activation(sst[:, 0:4], sst[:, 0:4], AF.Ln), r=sstk, w=sstk)
            P.act(lambda e: e.activation(sst[:, 4:8], sst[:, 0:4], AF.Exp, scale=-0.5), r=sstk, w=['rstd4'])
            P.dve(lambda e: e.tensor_tensor(
                ob[b][:].rearrange("p (g c) -> p g c", g=4), ysb[b][:].rearrange("p (g c) -> p g c", g=4),
                sst[:, 4:8].unsqueeze(2).to_broadcast([128, 4, 512]), ALU.mult), r=ytoks + ['rstd4'], w=['ob%d' % b])
            P.dma('sp', Dm['ossd'][r0:r0 + 128, :], ob[b][:], r=['ob%d' % b])

        stage1(0)
        for c in range(NT):
            if c + 1 < NT:
                stage1(c + 1)
            stage2(c)
        P.flush()


def load_weight(C, st, name, w_d, kchunks, ncols, stg=None, defer=False):
    P = C.P
    t = C.sbt(st, name, [128, kchunks, ncols], BF16)
    if defer:
        return t, (lambda: _load_weight_into(C, t, name, w_d, kchunks, stg))
    _load_weight_into(C, t, name, w_d, kchunks, stg)
    return t


def _load_weight_into(C, t, name, w_d, kchunks, stg):
    P = C.P
    wv = w_d.rearrange("(k p) c -> p k c", p=128)
    if stg is None:
        for k0 in range(0, kchunks, 4):
            P.dma('pool', t[:, k0:k0 + 4, :], wv[:, k0:k0 + 4, :], wa=[name])
        return
    for k0 in range(0, kchunks, 2):
        j = C.nstg % len(stg)
        C.nstg += 1
        P.dma('sp', stg[j][:], wv[:, k0:k0 + 2, :], w=['wstg%d' % j])
        dst = t[:, k0:k0 + 2, :]
        if C.nstg % 2 == 0:
            P.act(lambda e, j=j, dst=dst: e.copy(dst, stg[j][:]), r=['wstg%d' % j], wa=[name])
        else:
            P.dve(lambda e, j=j, dst=dst: e.tensor_copy(dst, stg[j][:]), r=['wstg%d' % j], wa=[name])


def stage_merge(C):
    nc, P, I, Dm = C.nc, C.P, C.I, C.D
    with ExitStack() as st:
        ngc = C.ngc
        sbt = lambda n, s, d: C.sbt(st, n, s, d)
        pst = lambda n, s, d: C.pst(st, n, s, d)
        wst = [sbt('e_wst%d' % i, [128, 2, D], F32) for i in range(2)]
        w_sb = load_weight(C, st, 'e_wsb', I['w_sb'], 8, D, wst)
        w_ssd = load_weight(C, st, 'e_wssd', I['w_ssd'], 16, D, wst)
        for k in range(16):
            P.dve(lambda e, k=k: e.tensor_scalar(w_ssd[:, k, :], w_ssd[:, k, :], ngc[:, k:k + 1], None, ALU.mult),
                  r=['e_wssd', 'e_ngc'], w=['e_wssd'])
        w_mix = load_weight(C, st, 'e_wmix', I['w_mix_out'], 8, D, wst)
        g_bc = sbt('e_g', [128, D], F32)
        b_bc = sbt('e_b', [128, D], F32)
        load_ln_params(C, I['ln1_g'], I['ln1_b'], g_bc, b_bc)
        osbT = sbt('e_osbT', [128, 8, 512], BF16)
        gT = sbt('e_gT', [128, 16, 512], BF16)
        ossd = sbt('e_ossd', [128, 4, 2048], BF16)
        ossdT = sbt('e_ossdT', [128, 16, 512], BF16)
        h0t = [sbt('e_h0%d' % i, [128, D], F32) for i in range(2)]
        mT = sbt('e_mT', [128, 8, 512], BF16)
        ta = [sbt('e_ta%d' % i, [128, 512], F32) for i in range(2)]
        tb = [sbt('e_tb%d' % i, [128, 512], F32) for i in range(2)]
        res = [sbt('e_res%d' % i, [128, D], F32) for i in range(2)]
        h1t = [sbt('e_h1%d' % i, [128, D], F32) for i in range(2)]
        stt = [sbt('e_st%d' % i, [128, 8], F32) for i in range(2)]
        junk = sbt('e_junk', [128, D], BF16)
        tp = [pst('e_tp%d' % i, [128, 8, 128], BF16) for i in range(2)]
        A_ps = [pst('e_A%d' % i, [128, 512], F32) for i in range(2)]
        B_ps = [pst('e_B%d' % i, [128, 512], F32) for i in range(2)]
        mix_ps = pst('e_mix', [128, D], F32)
        osbv = Dm['osbT'].rearrange("(k p) t -> p k t", p=128)
        gTv = Dm['gT'].rearrange("(k p) t -> p k t", p=128)
        ntp = 0
        nn = 0
        nres = 0
        def e_loads(tg):
            t0 = tg * 512
            P.dma('sp', osbT[:], osbv[:, :, t0:t0 + 512], w=['osbT'])
            P.dma('sp', gT[:], gTv[:, :, t0:t0 + 512], w=['gT'])
            P.dma('sp', ossd[:], Dm['ossd'][t0:t0 + 512, :].rearrange("(t p) c -> p t c", p=128), w=['ossd'])

        e_loads(0)
        for tg in range(8):
            t0 = tg * 512
            for tt in range(4):
                for hf in range(2):
                    pb = ntp % 2
                    ntp += 1
                    for j in range(8):
                        ch = hf * 8 + j
                        P.pe(lambda e, pb=pb, j=j, tt=tt, ch=ch: e.transpose(tp[pb][:, j, :], ossd[:, tt, ch * 128:(ch + 1) * 128], C.ident),
                             r=['ossd'], w=['tp%d' % pb])
                    if pb == 0:
                        P.act(lambda e, pb=pb, hf=hf, tt=tt: e.copy(ossdT[:, hf * 8:(hf + 1) * 8, tt * 128:(tt + 1) * 128], tp[pb][:]),
                              w=['tp%d' % pb], wa=['ossdT'])
                    else:
                        P.dve(lambda e, pb=pb, hf=hf, tt=tt: e.tensor_copy(ossdT[:, hf * 8:(hf + 1) * 8, tt * 128:(tt + 1) * 128], tp[pb][:]),
                              w=['tp%d' % pb], wa=['ossdT'])
            for nck in range(8):
                pb = nn % 2
                nn += 1
                for k in range(8):
                    P.pe(lambda e, pb=pb, k=k, nck=nck: e.matmul(A_ps[pb][:], w_sb[:, k, nck * 128:(nck + 1) * 128], osbT[:, k, :],
                                                                 start=(k == 0), stop=(k == 7)), r=['e_wsb', 'osbT'], w=['A%d' % pb])
                for k in range(16):
                    P.pe(lambda e, pb=pb, k=k, nck=nck: e.matmul(B_ps[pb][:], w_ssd[:, k, nck * 128:(nck + 1) * 128], ossdT[:, k, :],
                                                                 start=(k == 0), stop=(k == 15)), r=['e_wssd', 'ossdT'], w=['B%d' % pb])
                P.dve(lambda e, pb=pb, nck=nck: e.tensor_tensor(ta[pb][:], A_ps[pb][:], gT[:, nck, :], ALU.mult),
                      r=['gT'], w=['A%d' % pb, 'ta%d' % pb])
                P.dve(lambda e, pb=pb, nck=nck: e.tensor_tensor(tb[pb][:], B_ps[pb][:], gT[:, 8 + nck, :], ALU.mult),
                      r=['gT'], w=['B%d' % pb, 'tb%d' % pb])
                P.pool(lambda e, pb=pb, nck=nck: e.tensor_tensor(mT[:, nck, :], ta[pb][:], tb[pb][:], ALU.add),
                       r=['ta%d' % pb, 'tb%d' % pb], w=['mT'])
            if tg + 1 < 8:
                e_loads(tg + 1)
            for tt in range(4):
                rb = nres % 2
                nres += 1
                r0 = t0 + tt * 128
                P.dma('sp', h0t[rb][:], Dm['h0'][r0:r0 + 128, :], w=['h0t%d' % rb])
                for hf in range(2):
                    for k in range(8):
                        P.pe(lambda e, k=k, tt=tt, hf=hf: e.matmul(mix_ps[:, hf * 512:(hf + 1) * 512], mT[:, k, tt * 128:(tt + 1) * 128],
                                                                   w_mix[:, k, hf * 512:(hf + 1) * 512], start=(k == 0), stop=(k == 7)),
                             r=['mT', 'e_wmix'], w=['mix'])
                P.dve(lambda e, rb=rb, tt=tt: e.scalar_tensor_tensor(res[rb][:], h0t[rb][:], ALPHA, mix_ps[:], ALU.mult, ALU.add),
                      r=['h0t%d' % rb], w=['mix', 'res%d' % rb])
                ln_tile(C, res[rb][:], 'res%d' % rb, h1t[rb][:], 'h1t%d' % rb, g_bc[:], b_bc[:], stt[rb], junk[:], 'e%d' % rb)
                r0 = t0 + tt * 128
                P.dma('sp', Dm['h1'][r0:r0 + 128, :], h1t[rb][:], r=['h1t%d' % rb])
        P.flush()


def stage_xattn(C):
    nc, P, I, Dm = C.nc, C.P, C.I, C.D
    with ExitStack() as st:
        sbt = lambda n, s, d: C.sbt(st, n, s, d)
        pst = lambda n, s, d: C.pst(st, n, s, d)
        wst = [sbt('f_wst%d' % i, [128, 2, D], F32) for i in range(4)]
        w_xq, ld_xq = load_weight(C, st, 'f_wq', I['w_xq'], 8, D, wst, defer=True)
        w_xo, ld_xo = load_weight(C, st, 'f_wo', I['w_xo'], 8, D, wst, defer=True)
        KT = sbt('f_KT', [128, 8, MEM], BF16)
        Vm = sbt('f_V', [128, 2, D], BF16)
        g_bc = sbt('f_g', [128, D], F32)
        b_bc = sbt('f_b', [128, D], F32)
        load_ln_params(C, I['ln2_g'], I['ln2_b'], g_bc, b_bc)
        tpB = pst('f_tp', [128, 8, 128], BF16)
        q_ps = pst('f_qps', [128, 512], F32)
        s_ps = pst('f_sps', [128, 4, MEM], F32)
        OT_ps = pst('f_OTps', [128, 8, 128], F32)
        xa_ps = pst('f_xaps', [128, D], F32)
        with ExitStack() as pro:
            w_xk = load_weight(C, pro, 'f_wk', I['w_xk'], 8, D, wst)
            w_xv = load_weight(C, pro, 'f_wv', I['w_xv'], 8, D, wst)
            ld_xq()
            ld_xo()
            memf = C.sbt(pro, 'f_memf', [128, D], F32)
            memb = C.sbt(pro, 'f_memb', [128, D], BF16)
            memT = C.sbt(pro, 'f_memT', [128, 8, MEM], BF16)
            for mt in range(2):
                P.dma('sp', memf[:], I['mem'][mt * 128:(mt + 1) * 128, :], w=['memf'])
                P.act(lambda e: e.copy(memb[:], memf[:]), r=['memf'], w=['memb'])
                for c in range(8):
                    P.pe(lambda e, c=c: e.transpose(tpB[:, c, :], memb[:, c * 128:(c + 1) * 128], C.ident), r=['memb'], w=['tpB'])
                P.dve(lambda e, mt=mt: e.tensor_copy(memT[:, :, mt * 128:(mt + 1) * 128], tpB[:]), w=['tpB', 'memT'])
            for cc in range(8):
                for k in range(8):
                    P.pe(lambda e, cc=cc, k=k: e.matmul(q_ps[:, 0:MEM], w_xk[:, k, cc * 128:(cc + 1) * 128], memT[:, k, :],
                                                        start=(k == 0), stop=(k == 7)), r=['f_wk', 'memT'], w=['qps'])
                P.act(lambda e, cc=cc: e.copy(KT[:, cc, :], q_ps[:, 0:MEM]), w=['qps', 'KT'])
            for mt in range(2):
                for hf in range(2):
                    for k in range(8):
                        P.pe(lambda e, mt=mt, hf=hf, k=k: e.matmul(xa_ps[:, hf * 512:(hf + 1) * 512], memT[:, k, mt * 128:(mt + 1) * 128],
                                                                   w_xv[:, k, hf * 512:(hf + 1) * 512], start=(k == 0), stop=(k == 7)),
                             r=['f_wv', 'memT'], w=['xaps'])
                P.act(lambda e, mt=mt: e.copy(Vm[:, mt, :], xa_ps[:]), w=['xaps', 'Vm'])
            P.flush()
        h1f = [sbt('f_h1f%d' % i, [128, 4, D], F32) for i in range(2)]
        h1b = sbt('f_h1b', [128, D], BF16)
        h1T = [sbt('f_h1T%d' % i, [128, 8, 512], BF16) for i in range(2)]
        QT = [sbt('f_QT%d' % i, [128, 8, 512], BF16) for i in range(2)]
        pex = [sbt('f_pex%d' % i, [128, 4, MEM], F32) for i in range(2)]
        pn = [sbt('f_pn%d' % i, [128, 4 * MEM], BF16) for i in range(2)]
        PT = [sbt('f_PT%d' % i, [128, 8, 128], BF16) for i in range(2)]
        OT = sbt('f_OT', [128, 8, 128], BF16)
        sm = [sbt('f_sm%d' % i, [128, 16], F32) for i in range(2)]
        res = [sbt('f_res%d' % i, [128, D], F32) for i in range(2)]
        h2t = [sbt('f_h2%d' % i, [128, D], F32) for i in range(2)]
        stt = [sbt('f_st%d' % i, [128, 8], F32) for i in range(2)]
        junk = sbt('f_junk', [128, D], BF16)

        def prep(tg):
            gb = tg % 2
            t0 = tg * 512
            P.dma('sp', h1f[gb][:], Dm['h1'][t0:t0 + 512, :].rearrange("(t p) c -> p t c", p=128), w=['h1f%d' % gb])
            for tt in range(4):
                P.act(lambda e, tt=tt: e.copy(h1b[:], h1f[gb][:, tt, :]), r=['h1f%d' % gb], w=['h1b'])
                for c in range(8):
                    P.pe(lambda e, c=c: e.transpose(tpB[:, c, :], h1b[:, c * 128:(c + 1) * 128], C.ident), r=['h1b'], w=['tpB'])
                P.dve(lambda e, tt=tt: e.tensor_copy(h1T[gb][:, :, tt * 128:(tt + 1) * 128], tpB[:]), w=['tpB', 'h1T%d' % gb])
            for cc in range(8):
                for k in range(8):
                    P.pe(lambda e, cc=cc, k=k: e.matmul(q_ps[:], w_xq[:, k, cc * 128:(cc + 1) * 128], h1T[gb][:, k, :],
                                                        start=(k == 0), stop=(k == 7)), r=['f_wq', 'h1T%d' % gb], w=['qps'])
                P.act(lambda e, cc=cc: e.mul(QT[gb][:, cc, :], q_ps[:], 1.0 / 16.0), w=['qps', 'QT%d' % gb])

        def s1(ti):
            tg, tt = ti // 4, ti % 4
            gb = tg % 2
            rb = ti % 2
            ts = slice(tt * 128, (tt + 1) * 128)
            for h in range(4):
                for kk in range(2):
                    P.pe(lambda e, h=h, kk=kk: e.matmul(s_ps[:, h, :], QT[gb][:, 2 * h + kk, ts], KT[:, 2 * h + kk, :],
                                                        start=(kk == 0), stop=(kk == 1)), r=['QT%d' % gb, 'KT'], w=['sps'])
            smt = sm[rb]
            P.dve(lambda e: e.tensor_reduce(smt[:, 0:4], s_ps[:], axis=AX.X, op=ALU.max), w=['sps', 'mx%d' % rb])
            P.dve(lambda e: e.tensor_scalar(smt[:, 4:8], smt[:, 0:4], -1.0, None, ALU.mult), r=['mx%d' % rb], w=['nmx%d' % rb])
            P.pool(lambda e: e.memset(smt[:, 8:12], 0.0), w=['sum%d%d' % (rb, h) for h in range(4)])
            for h in range(4):
                P.act(lambda e, h=h: e.activation(pex[rb][:, h, :], s_ps[:, h, :], AF.Exp, bias=smt[:, 4 + h:5 + h],
                                                  accum_out=smt[:, 8 + h:9 + h]),
                      r=['nmx%d' % rb], w=['sps', 'pex%d' % rb, 'sum%d%d' % (rb, h)])
            sumt = ['sum%d%d' % (rb, h) for h in range(4)]
            P.dve(lambda e: e.reciprocal(smt[:, 12:16], smt[:, 8:12]), r=sumt, w=['rs%d' % rb])
            P.dve(lambda e: e.tensor_tensor(pn[rb][:].rearrange("p (h m) -> p h m", h=4), pex[rb][:],
                                            smt[:, 12:16].unsqueeze(2).to_broadcast([128, 4, MEM]), ALU.mult),
                  r=['pex%d' % rb, 'rs%d' % rb], w=['pn%d' % rb])
            for j in range(8):
                P.pe(lambda e, j=j: e.transpose(tpB[:, j, :], pn[rb][:, j * 128:(j + 1) * 128], C.ident), r=['pn%d' % rb], w=['tpB'])
            P.dve(lambda e: e.tensor_copy(PT[rb][:], tpB[:]), w=['tpB', 'PT%d' % rb])

        def s2(ti):
            tg, tt = ti // 4, ti % 4
            gb = tg % 2
            rb = ti % 2
            for cc in range(8):
                h = cc // 2
                for mt in range(2):
                    P.pe(lambda e, cc=cc, h=h, mt=mt: e.matmul(OT_ps[:, cc, :], Vm[:, mt, cc * 128:(cc + 1) * 128], PT[rb][:, 2 * h + mt, :],
                                                               start=(mt == 0), stop=(mt == 1)), r=['Vm', 'PT%d' % rb], w=['OTps'])
            P.act(lambda e: e.copy(OT[:], OT_ps[:]), w=['OTps', 'OT'])
            for hf in range(2):
                for cc in range(8):
                    P.pe(lambda e, hf=hf, cc=cc: e.matmul(xa_ps[:, hf * 512:(hf + 1) * 512], OT[:, cc, :], w_xo[:, cc, hf * 512:(hf + 1) * 512],
                                                          start=(cc == 0), stop=(cc == 7)), r=['OT', 'f_wo'], w=['xaps'])
            P.dve(lambda e: e.scalar_tensor_tensor(res[rb][:], h1f[gb][:, tt, :], ALPHA, xa_ps[:], ALU.mult, ALU.add),
                  r=['h1f%d' % gb], w=['xaps', 'res%d' % rb])
            ln_tile(C, res[rb][:], 'res%d' % rb, h2t[rb][:], 'h2t%d' % rb, g_bc[:], b_bc[:], stt[rb], junk[:], 'f%d' % rb)
            P.dma('sp', Dm['h2'][ti * 128:(ti + 1) * 128, :], h2t[rb][:], r=['h2t%d' % rb])

        prep(0)
        for ti in range(NT + 1):
            if ti < NT:
                s1(ti)
                if ti % 4 == 1 and ti // 4 + 1 < 8:
                    prep(ti // 4 + 1)
            if ti >= 1:
                s2(ti - 1)
        P.flush()


CAP = 1024
NSLOT = NE * CAP
XROW = 1032


def stage_moe(C):
    nc, P, I, Dm = C.nc, C.P, C.I, C.D
    xrows_d = C.xrows_d
    yrows_d = C.yrows_d
    with ExitStack() as st:
        bgc, buc = C.bgc, C.buc
        NW = 5
        W = [C.sbt(st, 'g_W%d' % i, [128, 8, D], BF16) for i in range(NW)]
        stg32 = [C.sbt(st, 'g_stg%d' % i, [128, 2, D], F32) for i in range(4)]
        desti = C.sbt(st, 'g_desti', [128, NT, 4], mybir.dt.int32)
        nw = [0]
        nst = [0]

        def loadW(src):
            i = nw[0] % NW
            nw[0] += 1
            wv = src.rearrange("(k p) c -> p k c", p=128)
            for k0 in (0, 2, 4, 6):
                j = nst[0] % 4
                nst[0] += 1
                P.dma('sp', stg32[j][:], wv[:, k0:k0 + 2, :], w=['stg32%d' % j])
                dst = W[i][:, k0:k0 + 2, :]
                if j == 0:
                    P.act(lambda e, j=j, dst=dst: e.copy(dst, stg32[j][:]), r=['stg32%d' % j], wa=['W%d' % i])
                elif j == 2:
                    P.act(lambda e, j=j, dst=dst: e.copy(dst, stg32[j][:]), r=['stg32%d' % j], wa=['W%d' % i])
                else:
                    P.dve(lambda e, j=j, dst=dst: e.tensor_copy(dst, stg32[j][:]), r=['stg32%d' % j], wa=['W%d' % i])
            return i

        wq = {'items': [], 'dma_i': 0, 'cast_i': 0}

        def queueW(src):
            i = nw[0] % NW
            nw[0] += 1
            wv = src.rearrange("(k p) c -> p k c", p=128)
            for k0 in (0, 2, 4, 6):
                wq['items'].append((wv[:, k0:k0 + 2, :], W[i][:, k0:k0 + 2, :], 'W%d' % i))
            return i

        def pumpW(n=1):
            for _ in range(n):
                while wq['dma_i'] < len(wq['items']) and wq['dma_i'] < wq['cast_i'] + 3:
                    srcq, _, _ = wq['items'][wq['dma_i']]
                    j = wq['dma_i'] % 4
                    P.dma('sp', stg32[j][:], srcq, w=['stg32%d' % j])
                    wq['dma_i'] += 1
                if wq['cast_i'] < wq['dma_i']:
                    _, dst, tok = wq['items'][wq['cast_i']]
                    j = wq['cast_i'] % 4
                    if j % 2 == 0:
                        P.act(lambda e, j=j, dst=dst: e.copy(dst, stg32[j][:]), r=['stg32%d' % j], wa=[tok])
                    else:
                        P.dve(lambda e, j=j, dst=dst: e.tensor_copy(dst, stg32[j][:]), r=['stg32%d' % j], wa=[tok])
                    wq['cast_i'] += 1

        with ExitStack() as ro:
            sbt = lambda n, s, d: C.sbt(ro, n, s, d)
            pst = lambda n, s, d: C.pst(ro, n, s, d)
            wr = sbt('r_w', [128, 8, NE], F32)
            brb = sbt('r_b', [128, NE], F32)
            P.dma('sp', wr[:], I['w_router'].rearrange("(k p) c -> p k c", p=128), w=['wr'])
            P.dma('sp', brb[:], I['b_router'].partition_broadcast(128), w=['brb'])
            h2f = [sbt('r_h2f%d' % i, [128, D], F32) for i in range(2)]
            h2Tf = [sbt('r_h2T%d' % i, [128, 8, 128], F32) for i in range(2)]
            xr = [sbt('r_xr%d' % i, [128, 4, XROW], BF16) for i in range(2)]
            tpf = [pst('r_tp%d' % i, [128, 8, 128], F32) for i in range(2)]
            lg_ps = [pst('r_lg%d' % i, [128, 512], F32) for i in range(2)]
            rk_ps = [pst('r_rk%d' % i, [128, 512], F32) for i in range(2)]
            lg = [sbt('r_lgs%d' % i, [128, NE], F32) for i in range(2)]
            mx8 = [sbt('r_mx%d' % i, [128, 8], F32) for i in range(2)]
            ix8 = [sbt('r_ix%d' % i, [128, 8], mybir.dt.uint32) for i in range(2)]
            ekf = [sbt('r_ek%d' % i, [128, 4], F32) for i in range(2)]
            e4 = [sbt('r_e4%d' % i, [128, 4], F32) for i in range(2)]
            p4 = [sbt('r_p4%d' % i, [128, 4], F32) for i in range(2)]
            sm = [sbt('r_sm%d' % i, [128, 4], F32) for i in range(2)]
            msk = [sbt('r_msk%d' % i, [128, NE], BF16) for i in range(2)]
            macc = sbt('r_macc', [128, NE], BF16)
            oh = [sbt('r_oh%d' % i, [128, NE], F32) for i in range(2)]
            rkk = [sbt('r_rkk%d' % i, [128, 4], F32) for i in range(2)]
            dstf = [sbt('r_dstf%d' % i, [128, 4], F32) for i in range(2)]
            ovf = [sbt('r_ovf%d' % i, [128, 4], F32) for i in range(2)]
            ovt = [sbt('r_ovt%d' % i, [128, 4], F32) for i in range(2)]
            P.dve(lambda e: e.memset(macc[:], 0.0), w=['macc'])
            tokf = [sbt('r_tokf%d' % i, [128, 1], F32) for i in range(2)]
            toki = [sbt('r_toki%d' % i, [128, 1], mybir.dt.int32) for i in range(2)]
            for i in range(2):
                P.dve(lambda e, i=i: e.memset(xr[i][:], 0.0), w=['xr%d' % i])
            iota = C.idf[:, 128:160]
            C.first_w = [loadW(I['w_e_gate'][0]), loadW(I['w_e_up'][0]), loadW(I['w_e_down'][0])]
            def tok_a(ti):
                b = ti % 2
                P.dma('sp', h2f[b][:], Dm['h2'][ti * 128:(ti + 1) * 128, :], w=['h2f%d' % b])
                for c in range(8):
                    P.pe(lambda e, b=b, c=c: e.transpose(tpf[b][:, c, :], h2f[b][:, c * 128:(c + 1) * 128], C.idf[:, 0:128]),
                         r=['h2f%d' % b, 'idf'], w=['tpf%d' % b])
                P.act(lambda e, b=b: e.copy(h2Tf[b][:], tpf[b][:]), w=['tpf%d' % b, 'h2Tf%d' % b])
                for k in range(8):
                    P.pe(lambda e, b=b, k=k: e.matmul(lg_ps[b][:, 0:NE], h2Tf[b][:, k, :], wr[:, k, :], start=(k == 0), stop=(k == 7)),
                         r=['h2Tf%d' % b, 'wr'], w=['lgps%d' % b])
                P.dve(lambda e, b=b: e.tensor_tensor(lg[b][:], lg_ps[b][:, 0:NE], brb[:], ALU.add), r=['brb'], w=['lgps%d' % b, 'lg%d' % b])
                P.dve(lambda e, b=b: e.max(mx8[b][:], lg[b][:]), r=['lg%d' % b], w=['mx8%d' % b])
                P.dve(lambda e, b=b: e.max_index(ix8[b][:], mx8[b][:], lg[b][:]), r=['lg%d' % b, 'mx8%d' % b], w=['ix8%d' % b])
                P.dve(lambda e, b=b: e.tensor_copy(ekf[b][:], ix8[b][:, 0:4]), r=['ix8%d' % b], w=['ekf%d' % b])
                P.dve(lambda e, b=b: e.tensor_scalar(msk[b][:], lg[b][:], mx8[b][:, 3:4], None, ALU.is_ge),
                      r=['lg%d' % b, 'mx8%d' % b], w=['msk%d' % b])
                P.dve(lambda e, b=b: e.tensor_scalar(sm[b][:, 0:1], mx8[b][:, 0:1], -1.0, None, ALU.mult), r=['mx8%d' % b], w=['nm%d' % b])
                P.act(lambda e, b=b: e.activation(e4[b][:], mx8[b][:, 0:4], AF.Exp, bias=sm[b][:, 0:1]), r=['mx8%d' % b, 'nm%d' % b], w=['e4%d' % b])
                P.dve(lambda e, b=b: e.reduce_sum(sm[b][:, 1:2], e4[b][:], axis=AX.X), r=['e4%d' % b], w=['sum%d' % b])
                P.dve(lambda e, b=b: e.reciprocal(sm[b][:, 2:3], sm[b][:, 1:2]), r=['sum%d' % b], w=['rs%d' % b])
                P.dve(lambda e, b=b: e.tensor_scalar(p4[b][:], e4[b][:], sm[b][:, 2:3], None, ALU.mult), r=['e4%d' % b, 'rs%d' % b], w=['p4%d' % b])
            def tok_b(ti):
                b = ti % 2
                P.pe(lambda e, b=b: e.matmul(rk_ps[b][:, 0:NE], C.cs('mlt'), msk[b][:], start=True, stop=False), r=['cst', 'msk%d' % b], w=['rkps%d' % b])
                P.pe(lambda e, b=b: e.matmul(rk_ps[b][:, 0:NE], C.cs('ones'), macc[:], start=False, stop=True), r=['cst', 'macc'], w=['rkps%d' % b])
                P.pool(lambda e, b=b: e.tensor_tensor(macc[:], macc[:], msk[b][:], ALU.add), r=['macc', 'msk%d' % b], w=['macc'])
                for k in range(4):
                    P.dve(lambda e, b=b, k=k: e.tensor_scalar(oh[b][:], iota, ekf[b][:, k:k + 1], None, ALU.is_equal),
                          r=['idf', 'ekf%d' % b], w=['oh%d' % b])
                    P.dve(lambda e, b=b: e.tensor_tensor(oh[b][:], oh[b][:], rk_ps[b][:, 0:NE], ALU.mult), r=['oh%d' % b], w=['oh%d' % b, 'rkps%d' % b])
                    P.dve(lambda e, b=b, k=k: e.reduce_sum(rkk[b][:, k:k + 1], oh[b][:], axis=AX.X), r=['oh%d' % b], w=['rkk%d%d' % (b, k)])
                rkt = ['rkk%d%d' % (b, k) for k in range(4)]
                P.dve(lambda e, b=b: e.scalar_tensor_tensor(dstf[b][:], ekf[b][:], float(CAP), rkk[b][:], ALU.mult, ALU.add),
                      r=rkt + ['ekf%d' % b], w=['dstf%d' % b])
                P.dve(lambda e, b=b: e.tensor_scalar(ovf[b][:], rkk[b][:], float(CAP) - 0.5, None, ALU.is_ge), r=rkt, w=['ovf%d' % b])
                P.dve(lambda e, b=b: e.tensor_scalar(ovt[b][:], dstf[b][:], -1.0, float(NSLOT), ALU.mult, ALU.add), r=['dstf%d' % b], w=['ovt%d' % b])
                P.dve(lambda e, b=b: e.tensor_tensor(ovt[b][:], ovt[b][:], ovf[b][:], ALU.mult), r=['ovt%d' % b, 'ovf%d' % b], w=['ovt%d' % b])
                P.dve(lambda e, b=b: e.tensor_tensor(dstf[b][:], dstf[b][:], ovt[b][:], ALU.add), r=['ovt%d' % b, 'dstf%d' % b], w=['dstf%d' % b])
                P.dve(lambda e, b=b, ti=ti: e.tensor_copy(desti[:, ti, :], dstf[b][:]), r=['dstf%d' % b], w=['desti%d' % ti])
                P.dve(lambda e, b=b, ti=ti: e.tensor_scalar(tokf[b][:], C.idf[:, 160:161], float(ti * 128), None, ALU.add),
                      r=['idf'], w=['tokf%d' % b])
                P.dve(lambda e, b=b: e.tensor_copy(toki[b][:], tokf[b][:]), r=['tokf%d' % b], w=['toki%d' % b])
                P.act(lambda e, b=b: e.copy(xr[b][:, 0, 0:D], h2f[b][:]), r=['h2f%d' % b], w=['xrx%d' % b], wa=['xr%d' % b])
                for k in range(1, 4):
                    P.dve(lambda e, b=b, k=k: e.tensor_copy(xr[b][:, k, 0:D], xr[b][:, 0, 0:D]), r=['xrx%d' % b], wa=['xr%d' % b])
                for k in range(4):
                    P.dve(lambda e, b=b, k=k: e.tensor_copy(xr[b][:, k, D:D + 2].bitcast(F32), p4[b][:, k:k + 1]),
                          r=['p4%d' % b], wa=['xr%d' % b])
                    P.dve(lambda e, b=b, k=k: e.tensor_copy(xr[b][:, k, D + 2:D + 4].bitcast(mybir.dt.int32), toki[b][:]),
                          r=['toki%d' % b], wa=['xr%d' % b])
                for k in range(4):
                    P.op('pool', lambda e, b=b, k=k, ti=ti: e.indirect_dma_start(
                        out=xrows_d[:, :], out_offset=bass.IndirectOffsetOnAxis(ap=desti[:, ti, k:k + 1], axis=0),
                        in_=xr[b][:, k, :], in_offset=None),
                        ['xr%d' % b, 'desti%d' % ti], ['xrows_%d_%d' % (ti, k)], dma=True)
            tok_a(0)
            for ti in range(NT):
                if ti + 1 < NT:
                    tok_a(ti + 1)
                tok_b(ti)
            P.flush()
        with ExitStack() as ex_:
            sbt = lambda n, s, d: C.sbt(ex_, n, s, d)
            pst = lambda n, s, d: C.pst(ex_, n, s, d)
            bd = [sbt('g_bd%d' % i, [1, D], BF16) for i in range(2)]
            xg = [sbt('g_xg%d' % i, [128, 4, XROW], BF16) for i in range(2)]
            xT = [sbt('g_xT%d' % i, [128, 8, 512], BF16) for i in range(2)]
            actT = [sbt('g_act%d' % i, [128, 8, 512], BF16) for i in range(2)]
            gc = [sbt('g_gc%d' % i, [128, 512], F32) for i in range(2)]
            sg = [sbt('g_sg%d' % i, [128, 512], F32) for i in range(2)]
            u1 = [sbt('g_u1%d' % i, [128, 512], F32) for i in range(2)]
            yw = [sbt('g_yw%d' % i, [128, D], F32) for i in range(2)]
            tp = [pst('g_tp%d' % i, [128, 8, 128], BF16) for i in range(2)]
            g_ps = [pst('g_gps%d' % i, [128, 512], F32) for i in range(2)]
            u_ps = [pst('g_ups%d' % i, [128, 512], F32) for i in range(2)]
            y_ps = [pst('g_yps%d' % i, [128, 512], F32) for i in range(2)]
            NG = CAP // 512
            groups = [(e_, gi) for e_ in range(NE) for gi in range(NG)]
            wsel = {0: tuple(C.first_w)}
            cnt = {'nf': 0, 'ny': 0, 'ntp': 0}

            def load_x(i):
                e_, gi = groups[i]
                base = e_ * CAP + gi * 512
                ab = i % 2
                P.dma('sp', xg[ab][:], xrows_d[base:base + 512, :].rearrange("(b p) c -> p b c", p=128), w=['xg%d' % ab])

            def phase_t(i):
                ab = i % 2
                for b4 in range(4):
                    tb = cnt['ntp'] % 2
                    cnt['ntp'] += 1
                    for c in range(8):
                        P.pe(lambda e, tb=tb, b4=b4, c=c: e.transpose(tp[tb][:, c, :], xg[ab][:, b4, c * 128:(c + 1) * 128], C.ident),
                             r=['xg%d' % ab], w=['tp%d' % tb])
                    if b4 % 2 == 0:
                        P.act(lambda e, tb=tb, b4=b4: e.copy(xT[ab][:, :, b4 * 128:(b4 + 1) * 128], tp[tb][:]), w=['tp%d' % tb], wa=['xT%d' % ab])
                    else:
                        P.dve(lambda e, tb=tb, b4=b4: e.tensor_copy(xT[ab][:, :, b4 * 128:(b4 + 1) * 128], tp[tb][:]), w=['tp%d' % tb], wa=['xT%d' % ab])

            def phase_gu(i):
                e_, gi = groups[i]
                ab = i % 2
                ig, iu, idn = wsel[e_]
                for fc in range(8):
                    fb = cnt['nf'] % 2
                    cnt['nf'] += 1
                    col = e_ * 8 + fc
                    for k in range(8):
                        P.pe(lambda e, fb=fb, k=k, fc=fc: e.matmul(
                            g_ps[fb][:], W[ig][:, k, fc * 128:(fc + 1) * 128], xT[ab][:, k, :], start=(k == 0), stop=(k == 7)),
                            r=['W%d' % ig, 'xT%d' % ab], w=['gps%d' % fb])
                    for k in range(8):
                        P.pe(lambda e, fb=fb, k=k, fc=fc: e.matmul(
                            u_ps[fb][:], W[iu][:, k, fc * 128:(fc + 1) * 128], xT[ab][:, k, :], start=(k == 0), stop=(k == 7)),
                            r=['W%d' % iu, 'xT%d' % ab], w=['ups%d' % fb])
                    P.dve(lambda e, fb=fb, col=col: e.tensor_scalar(gc[fb][:], g_ps[fb][:], bgc[:, col:col + 1], 7.0, ALU.add, ALU.min),
                          r=['g_bg'], w=['gps%d' % fb, 'gc%d' % fb])
                    P.act(lambda e, fb=fb: e.activation(sg[fb][:], gc[fb][:], AF.Sigmoid, scale=1.702), r=['gc%d' % fb], w=['sg%d' % fb])
                    P.dve(lambda e, fb=fb, col=col: e.tensor_scalar(u1[fb][:], u_ps[fb][:], buc[:, col:col + 1], 8.0, ALU.add, ALU.min),
                          r=['g_bu'], w=['ups%d' % fb, 'u1%d' % fb])
                    P.dve(lambda e, fb=fb: e.tensor_tensor(sg[fb][:], sg[fb][:], gc[fb][:], ALU.mult),
                          r=['sg%d' % fb, 'gc%d' % fb], w=['sg%d' % fb])
                    P.dve(lambda e, fb=fb, fc=fc: e.scalar_tensor_tensor(actT[ab][:, fc, :], u1[fb][:], -6.0, sg[fb][:], ALU.max, ALU.mult),
                          r=['u1%d' % fb, 'sg%d' % fb], w=['actT%d' % ab])
                    if gi == 0:
                        pumpW(1)

            def phase_d(i):
                e_, gi = groups[i]
                ab = i % 2
                bb = e_ % 2
                ig, iu, idn = wsel[e_]
                base = e_ * CAP + gi * 512
                for b4 in range(4):
                    wb_ = cnt['ny'] % 2
                    cnt['ny'] += 1
                    for hf in range(2):
                        for fc in range(8):
                            P.pe(lambda e, hf=hf, fc=fc, b4=b4: e.matmul(
                                y_ps[hf][:], actT[ab][:, fc, b4 * 128:(b4 + 1) * 128],
                                W[idn][:, fc, hf * 512:(hf + 1) * 512], start=(fc == 0), stop=False),
                                r=['actT%d' % ab, 'W%d' % idn], w=['yps%d' % hf])
                        P.pe(lambda e, hf=hf: e.matmul(
                            y_ps[hf][:], C.cs('ones')[0:1, :], bd[bb][0:1, hf * 512:(hf + 1) * 512],
                            start=False, stop=True), r=['cst', 'bd%d' % bb], w=['yps%d' % hf])
                        evac = P.act if hf == 0 else P.act
                        evac(lambda e, hf=hf, wb_=wb_, b4=b4: e.activation(
                            yw[wb_][:, hf * 512:(hf + 1) * 512], y_ps[hf][:], AF.Identity, scale=xg[ab][:, b4, D:D + 2].bitcast(F32)),
                            r=['xg%d' % ab], w=['yps%d' % hf], wa=['yw%d' % wb_])
                    P.op('pool', lambda e, wb_=wb_, b4=b4: e.indirect_dma_start(
                        out=C.ff_d[:, :], out_offset=bass.IndirectOffsetOnAxis(ap=xg[ab][:, b4, D + 2:D + 4].bitcast(mybir.dt.int32), axis=0),
                        in_=yw[wb_][:], in_offset=None, compute_op=ALU.add),
                        ['yw%d' % wb_, 'xg%d' % ab], ['ffacc'], dma=True)
                    if gi == NG - 1:
                        pumpW(1)

            load_x(0)
            phase_t(0)
            for i, (e_, gi) in enumerate(groups):
                if gi == 0:
                    bb = e_ % 2
                    P.dma('pool', bd[bb][:], I['b_e_down'][e_:e_ + 1, :], w=['bd%d' % bb])
                    if e_ + 1 < NE:
                        nxt_gu = (queueW(I['w_e_gate'][e_ + 1]), queueW(I['w_e_up'][e_ + 1]))
                if i + 1 < len(groups):
                    load_x(i + 1)
                phase_gu(i)
                if gi == NG - 1 and e_ + 1 < NE:
                    pumpW(8)
                    wsel[e_ + 1] = (nxt_gu[0], nxt_gu[1], queueW(I['w_e_down'][e_ + 1]))
                if i + 1 < len(groups):
                    phase_t(i + 1)
                phase_d(i)
            P.flush()
        with ExitStack() as fin:
            sbt = lambda n, s, d: C.sbt(fin, n, s, d)
            g_bc = sbt('h_g', [128, D], F32)
            b_bc = sbt('h_b', [128, D], F32)
            load_ln_params(C, I['ln3_g'], I['ln3_b'], g_bc, b_bc)
            ffl = [sbt('h_ff%d' % i, [128, D], F32) for i in range(2)]
            h2 = [sbt('h_h2%d' % i, [128, D], F32) for i in range(2)]
            res = [sbt('h_res%d' % i, [128, D], F32) for i in range(2)]
            ot = [sbt('h_ot%d' % i, [128, D], F32) for i in range(2)]
            stt = [sbt('h_st%d' % i, [128, 8], F32) for i in range(2)]
            junk = sbt('h_junk', [128, D], BF16)
            def h_p1(ti):
                b = ti % 2
                rs = slice(ti * 128, (ti + 1) * 128)
                P.dma('sp', ffl[b][:], C.ff_d[rs, :], w=['ffl%d' % b])
                P.dma('sp', h2[b][:], Dm['h2'][rs, :], w=['h2%d' % b])
                P.dve(lambda e, b=b: e.scalar_tensor_tensor(res[b][:], h2[b][:], ALPHA, ffl[b][:], ALU.mult, ALU.add),
                      r=['ffl%d' % b, 'h2%d' % b], w=['res%d' % b])
                ln_stats(C, res[b][:], 'res%d' % b, stt[b], junk[:], 'h%d' % b)

            h_p1(0)
            for ti in range(NT):
                b = ti % 2
                rs = slice(ti * 128, (ti + 1) * 128)
                if ti + 1 < NT:
                    h_p1(ti + 1)
                ln_apply(C, res[b][:], 'res%d' % b, ot[b][:], 'ot%d' % b, g_bc[:], b_bc[:], stt[b], 'h%d' % b)
                P.dma('pool', C.out[rs, :], ot[b][:], r=['ot%d' % b])
            P.flush()


_NC_CACHE = {}


def get_program(stop_after=None, debug=False):
    key = (stop_after, debug)
    if key not in _NC_CACHE:
        _NC_CACHE[key] = build_program(stop_after, debug)
    return _NC_CACHE[key]


def make_in_maps(inputs, n_cores=8):
    f = lambda a: np.ascontiguousarray(np.asarray(a, dtype=np.float32))
    shared = {
        'cst_bf': CST_BF, 'cst_f32': CST_F32, 'cst_i32': CST_I32,
        'ln_in_g': f(inputs['ln_in_g']), 'ln_in_b': f(inputs['ln_in_b']),
        'w_in': f(inputs['w_in'])[0], 'b_branch_gate': f(inputs['b_branch_gate'])[0].reshape(-1),
        'conv_w': f(inputs['conv_w'])[0], 'conv_b': f(inputs['conv_b'])[0],
        'dt_bias': f(inputs['dt_bias'])[0], 'a_log': f(inputs['a_log'])[0], 'd_skip': f(inputs['d_skip'])[0],
        'ssd_norm_g': f(inputs['ssd_norm_g'])[0], 'w_sb': f(inputs['w_sb'])[0], 'w_ssd': f(inputs['w_ssd'])[0],
        'w_mix_out': f(inputs['w_mix_out'])[0], 'ln1_g': f(inputs['ln1_g'])[0], 'ln1_b': f(inputs['ln1_b'])[0],
        'w_xq': f(inputs['w_xq'])[0], 'w_xk': f(inputs['w_xk'])[0], 'w_xv': f(inputs['w_xv'])[0],
        'w_xo': f(inputs['w_xo'])[0], 'ln2_g': f(inputs['ln2_g'])[0], 'ln2_b': f(inputs['ln2_b'])[0],
        'w_router': f(inputs['w_router'])[0], 'b_router': f(inputs['b_router'])[0],
        'w_e_gate': f(inputs['w_e_gate'])[0], 'b_e_gate': f(inputs['b_e_gate'])[0].reshape(-1),
        'w_e_up': f(inputs['w_e_up'])[0], 'b_e_up': f(inputs['b_e_up'])[0].reshape(-1),
        'w_e_down': f(inputs['w_e_down'])[0], 'b_e_down': f(inputs['b_e_down'])[0],
        'ln3_g': f(inputs['ln3_g'])[0], 'ln3_b': f(inputs['ln3_b'])[0],
    }
    x = f(inputs['x'])
    mem = f(inputs['mem'])
    maps = []
    for c in range(n_cores):
        m = dict(shared)
        m['x'] = x[c]
        m['mem'] = mem[c]
        maps.append(m)
    return maps


def kernel(**inputs):
    nc = get_program()
    in_maps = make_in_maps(inputs, 8)
    res = run_bass_kernel_spmd(nc, in_maps, core_ids=list(range(8)))
    return np.stack([np.asarray(r['out'], dtype=np.float32) for r in res.results], axis=0)
```

```python
import numpy as np
import ml_dtypes
from contextlib import ExitStack
import concourse.bass as bass
import concourse.mybir as mybir
from concourse.bass_utils import run_bass_kernel_spmd

F32 = mybir.dt.float32
BF16 = mybir.dt.bfloat16
AF = mybir.ActivationFunctionType
ALU = mybir.AluOpType
AX = mybir.AxisListType

S = 4096
D = 1024
NT = S // 128
MEM = 256
NE = 32
ALPHA = 2.0 ** 0.25
EPS = 1e-5
IN_W = 10272

ENGS = ['pe', 'act', 'dve', 'pool', 'sp']
NDS = 8


class Prog:
    def __init__(self, nc, stack):
        self.nc = nc
        self.sem = {e: stack.enter_context(nc.semaphore('s_' + e)) for e in ENGS}
        self.dsem = {q: [stack.enter_context(nc.semaphore('d_%s%d' % (q, i))) for i in range(NDS)]
                     for q in ('sp', 'act', 'pool')}
        self.cnt = {e: 0 for e in ENGS}
        self.dcnt = {q: [0] * NDS for q in self.dsem}
        self.dnext = {q: 0 for q in self.dsem}
        self.reset_stage()

    def reset_stage(self):
        self.ops = {e: [] for e in ENGS}
        self.W = {}
        self.R = {}
        self.PR = {}
        self.XW = {}
        self.pos = {e: 0 for e in ENGS}

    def op(self, eng, fn, reads=(), writes=(), dma=False, wa=()):
        deps = []
        for t in reads:
            deps.extend(self.W.get(t, ()))
        for t in writes:
            deps.extend(self.W.get(t, ()))
            deps.extend(self.R.get(t, ()))
        for t in wa:
            if self.R.get(t):
                self.PR[t] = self.R[t]
                self.R[t] = []
                self.W[t] = []
                self.XW[t] = []
            deps.extend(self.PR.get(t, ()))
            deps.extend(self.XW.get(t, ()))
        o = {'eng': eng, 'fn': fn, 'deps': deps, 'dma': dma, 'signal': dma, 'pos': self.pos[eng]}
        self.pos[eng] += 1
        for d in deps:
            d['signal'] = True
        self.ops[eng].append(o)
        for t in reads:
            self.R.setdefault(t, []).append(o)
        for t in writes:
            self.W[t] = [o]
            self.XW[t] = [o]
            self.R[t] = []
            self.PR[t] = []
        for t in wa:
            self.W.setdefault(t, []).append(o)
        return o

    def pe(self, fn, r=(), w=(), wa=()):
        return self.op('pe', fn, r, w, wa=wa)

    def act(self, fn, r=(), w=(), wa=()):
        return self.op('act', fn, r, w, wa=wa)

    def dve(self, fn, r=(), w=(), wa=()):
        return self.op('dve', fn, r, w, wa=wa)

    def pool(self, fn, r=(), w=(), wa=()):
        return self.op('pool', fn, r, w, wa=wa)

    def dma(self, q, out, in_, r=(), w=(), wa=(), **kw):
        return self.op(q, lambda e: e.dma_start(out=out, in_=in_, **kw), r, w, dma=True, wa=wa)

    def flush(self):
        nc = self.nc
        for e in ENGS:
            cops = [o for o in self.ops[e] if not o['dma']]
            if cops:
                cops[-1]['signal'] = True
        for e in ENGS:
            c = self.cnt[e]
            for o in self.ops[e]:
                if o['dma']:
                    slot = self.dnext[e] % NDS
                    self.dnext[e] += 1
                    self.dcnt[e][slot] += 1
                    o['sig'] = (self.dsem[e][slot], 16 * self.dcnt[e][slot])
                    o['slotwait'] = (self.dsem[e][slot], 16 * (self.dcnt[e][slot] - 1))
                elif o['signal']:
                    c += 1
                    o['sig'] = (self.sem[e], c)
            self.cnt[e] = c
        end_c = dict(self.cnt)
        end_d = {q: list(v) for q, v in self.dcnt.items()}

        def emit(e, eng):
            waited = {}

            def wait(sem, val):
                k = id(sem)
                if val <= 0 or waited.get(k, 0) >= val:
                    return
                waited[k] = val
                eng.wait_ge(sem, val)

            for o in self.ops[e]:
                for d in o['deps']:
                    if d is o:
                        continue
                    if d['eng'] == e and not d['dma']:
                        if e == 'pe' or e == 'sp':
                            continue
                        if o['pos'] - d['pos'] > 6:
                            continue
                    wait(*d['sig'])
                if o['dma']:
                    wait(*o['slotwait'])
                ins = o['fn'](eng)
                if o['signal']:
                    s, v = o['sig']
                    ins.then_inc(s, 16 if o['dma'] else 1)
            for e2 in ENGS:
                if e2 != e and e2 != 'sp':
                    wait(self.sem[e2], end_c[e2])
            for q in self.dsem:
                for i in range(NDS):
                    wait(self.dsem[q][i], 16 * end_d[q][i])

        with nc.Block() as block:
            @block.tensor
            def _(eng):
                emit('pe', eng)

            @block.scalar
            def _(eng):
                emit('act', eng)

            @block.vector
            def _(eng):
                emit('dve', eng)

            @block.gpsimd
            def _(eng):
                emit('pool', eng)

            @block.sync
            def _(eng):
                emit('sp', eng)
        self.reset_stage()


def make_consts():
    j = np.arange(128)[:, None]
    c = np.arange(128)[None, :]
    cb = np.arange(896)[None, :]
    tabs = {
        'ident': (j == c),
        'negtri': -1.0 * (j >= c),
        'negones': -np.ones((128, 128)),
        'ones': np.ones((128, 128)),
        'mgt': (j > c),
        'mle': (j <= c),
        'mlt': (j < c),
        'mask01': (j + 384 < cb),
        'negbig': -30000.0 * (~(j + 384 < cb)),
    }
    offs = {}
    cols = []
    o = 0
    for k, v in tabs.items():
        v = np.asarray(v, dtype=np.float32)
        offs[k] = (o, v.shape[1])
        o += v.shape[1]
        cols.append(v)
    full = np.concatenate(cols, axis=1)
    return full.astype(ml_dtypes.bfloat16), offs


CST_BF, CST_OFF = make_consts()
CST_F32 = np.concatenate([np.eye(128, dtype=np.float32), np.tile(np.arange(32, dtype=np.float32)[None, :], (128, 1)),
                          np.arange(128, dtype=np.float32)[:, None]], axis=1)
CST_I32 = (S + (np.arange(128)[:, None] * 8 + np.arange(8)[None, :]) % 128).astype(np.int32)


class Ctx:
    pass


def build_program(stop_after=None, debug=False, only=None):
    nc = bass.Bass("TRN2", target_bir_lowering=False)
    C = Ctx()
    C.nc = nc
    C.debug = debug
    C.nstg = 0

    def din(name, shape, dt=F32):
        return nc.dram_tensor(name, list(shape), dt, kind="ExternalInput").ap()

    def dscr(name, shape, dt):
        return nc.dram_tensor(name, list(shape), dt, kind="ExternalOutput" if debug else "Internal").ap()

    I = {}
    I['x'] = din('x', [S, D])
    I['mem'] = din('mem', [MEM, D])
    I['cst_bf'] = din('cst_bf', list(CST_BF.shape), BF16)
    I['cst_f32'] = din('cst_f32', [128, 161])
    I['cst_i32'] = din('cst_i32', [128, 8], mybir.dt.int32)
    for nm, shp in [('ln_in_g', [D]), ('ln_in_b', [D]), ('w_in', [D, IN_W]), ('b_branch_gate', [2 * D]),
                    ('conv_w', [4, 3072]), ('conv_b', [3072]), ('dt_bias', [32]), ('a_log', [32]),
                    ('d_skip', [32]), ('ssd_norm_g', [2048]), ('w_sb', [D, D]), ('w_ssd', [2048, D]),
                    ('w_mix_out', [D, D]), ('ln1_g', [D]), ('ln1_b', [D]), ('w_xq', [D, D]), ('w_xk', [D, D]),
                    ('w_xv', [D, D]), ('w_xo', [D, D]), ('ln2_g', [D]), ('ln2_b', [D]), ('w_router', [D, NE]),
                    ('b_router', [NE]), ('w_e_gate', [NE, D, D]), ('b_e_gate', [NE * D]), ('w_e_up', [NE, D, D]),
                    ('b_e_up', [NE * D]), ('w_e_down', [NE, D, D]), ('b_e_down', [NE, D]), ('ln3_g', [D]),
                    ('ln3_b', [D])]:
        I[nm] = din(nm, shp)
    C.I = I
    out = nc.dram_tensor('out', [S, D], F32, kind="ExternalOutput").ap()
    C.out = out
    Dm = {}
    Dm['h0'] = dscr('h0_d', [S, D], F32)
    Dm['qT'] = dscr('qT_d', [D, S], BF16)
    Dm['kT'] = dscr('kT_d', [D, S], BF16)
    Dm['v'] = dscr('v_d', [S, D], BF16)
    Dm['z'] = dscr('z_d', [S, 2048], BF16)
    Dm['xs'] = dscr('xs_d', [S, 2048], BF16)
    Dm['btm'] = dscr('btm_d', [S, 512], BF16)
    Dm['bT'] = dscr('bT_d', [512, S], BF16)
    Dm['cT'] = dscr('cT_d', [512, S], BF16)
    Dm['gT'] = dscr('gT_d', [2048, S], BF16)
    Dm['osbT'] = dscr('osbT_d', [D, S], BF16)
    Dm['ossd'] = dscr('ossd_d', [S, 2048], BF16)
    Dm['h1'] = dscr('h1_d', [S, D], F32)
    Dm['h2'] = dscr('h2_d', [S, D], F32)
    C.xrows_d = nc.dram_tensor('xrows_d', [NE * 1024 + 128, 1032], BF16, kind='Internal').ap()
    C.yrows_d = nc.dram_tensor('yrows_d', [NE * 1024 + 128, D], F32, kind='Internal').ap()
    C.ff_d = nc.dram_tensor('ff_d', [S + 128, D], F32, kind='Internal').ap()
    C.D = Dm

    with ExitStack() as top:
        P = Prog(nc, top)
        C.P = P

        def sbt(st, name, shape, dt):
            return st.enter_context(nc.sbuf_tensor(name, list(shape), dt))

        def pst(st, name, shape, dt):
            return st.enter_context(nc.psum_tensor(name, list(shape), dt))

        C.sbt = sbt
        C.pst = pst
        cst = sbt(top, 'cst', list(CST_BF.shape), BF16)
        idf = sbt(top, 'idf', [128, 161], F32)
        C.cst = cst
        C.idf = idf

        def cs(name, lo=0, n=None):
            o, w = CST_OFF[name]
            if n is None:
                n = w
            return cst[:, o + lo:o + lo + n]

        C.cs = cs
        C.ident = cs('ident')
        C.dt_sb = sbt(top, 'dt_sb', [128, NT, 32], F32)
        C.da_bf = sbt(top, 'da_bf', [128, NT, 32], BF16)
        P.dma('sp', cst[:], I['cst_bf'], w=['cst'])
        P.dma('sp', idf[:], I['cst_f32'], w=['idf'])
        P.flush()

        C.ngc, C.bgc, C.buc = load_cols_multi(C, top, [(I['ssd_norm_g'], 16, 'e_ngc'), (I['b_e_gate'], NE * 8, 'g_bg'),
                                                       (I['b_e_up'], NE * 8, 'g_bu')])
        P.dve(lambda e: e.tensor_scalar(C.buc[:], C.buc[:], 1.0, None, ALU.add), w=['g_bu'])
        stages = [stage_ab, stage_attn, stage_ssd, stage_merge, stage_xattn, stage_moe]
        names = ['ab', 'attn', 'ssd', 'merge', 'xattn', 'moe']
        with ExitStack() as hts:
            for fn, nm in zip(stages, names):
                if only is not None and nm not in only:
                    continue
                fn(C)
                if stop_after == nm:
                    break
        if stop_after is not None:
            with ExitStack() as st:
                z = sbt(st, 'zout', [128, D], F32)
                P.dve(lambda e: e.memset(z[:], 0.0), w=['zout'])
                P.dma('sp', out[0:128, :], z[:], r=['zout'])
                P.flush()
    return nc


def ln_stats(C, src, src_tok, stt, junk, key):
    P = C.P
    s = key
    P.dve(lambda e: e.reduce_sum(stt[:, 0:1], src, axis=AX.X), r=[src_tok], w=[s + 's1'])
    P.pool(lambda e: e.memset(stt[:, 1:2], 0.0), w=[s + 's2'])
    P.act(lambda e: e.activation(junk, src, AF.Square, accum_out=stt[:, 1:2]), r=[src_tok], w=[s + 's2', s + 'junk'])
    P.dve(lambda e: e.tensor_scalar(stt[:, 2:3], stt[:, 0:1], 1.0 / D, None, ALU.mult), r=[s + 's1'], w=[s + 'mean'])
    P.dve(lambda e: e.tensor_tensor(stt[:, 3:4], stt[:, 2:3], stt[:, 2:3], ALU.mult), r=[s + 'mean'], w=[s + 'msq'])
    P.dve(lambda e: e.scalar_tensor_tensor(stt[:, 4:5], stt[:, 1:2], 1.0 / D, stt[:, 3:4], ALU.mult, ALU.subtract),
          r=[s + 's2', s + 'msq'], w=[s + 'var'])
    P.dve(lambda e: e.tensor_scalar(stt[:, 4:5], stt[:, 4:5], 0.0, EPS, ALU.max, ALU.add), r=[s + 'var'], w=[s + 'var'])
    P.act(lambda e: e.activation(stt[:, 5:6], stt[:, 4:5], AF.Ln), r=[s + 'var'], w=[s + 'lnv'])
    P.act(lambda e: e.activation(stt[:, 6:7], stt[:, 5:6], AF.Exp, scale=-0.5), r=[s + 'lnv'], w=[s + 'rstd'])
    P.dve(lambda e: e.scalar_tensor_tensor(stt[:, 7:8], stt[:, 2:3], -1.0, stt[:, 6:7], ALU.mult, ALU.mult),
          r=[s + 'mean', s + 'rstd'], w=[s + 'nmr'])


def ln_apply(C, src, src_tok, dst, dst_tok, g_bc, b_bc, stt, key):
    P = C.P
    s = key
    P.act(lambda e: e.activation(dst, src, AF.Identity, scale=stt[:, 6:7], bias=stt[:, 7:8]),
          r=[src_tok, s + 'rstd', s + 'nmr'], w=[dst_tok])
    P.dve(lambda e: e.tensor_tensor(dst, dst, g_bc, ALU.mult), r=[dst_tok, 'lnp'], w=[dst_tok])
    P.dve(lambda e: e.tensor_tensor(dst, dst, b_bc, ALU.add), r=[dst_tok, 'lnp'], w=[dst_tok])


def ln_tile(C, src, src_tok, dst, dst_tok, g_bc, b_bc, stt, junk, key):
    ln_stats(C, src, src_tok, stt, junk, key)
    ln_apply(C, src, src_tok, dst, dst_tok, g_bc, b_bc, stt, key)


def load_ln_params(C, g_d, b_d, g_bc, b_bc):
    C.P.dma('sp', g_bc[:], g_d.partition_broadcast(128), wa=['lnp'])
    C.P.dma('sp', b_bc[:], b_d.partition_broadcast(128), wa=['lnp'])


def load_cols_multi(C, st, specs):
    P = C.P
    outs = [C.sbt(st, name, [128, ncols], F32) for (_, ncols, name) in specs]
    with ExitStack() as tmp:
        tag = specs[0][2]
        ps = C.pst(tmp, 'lc_ps_' + tag, [128, 512], F32)
        nr = 0
        rows = [C.sbt(tmp, 'lc_rows_%s%d' % (tag, i), [128, 128], F32) for i in range(4)]
        for (vec_d, ncols, name), outt in zip(specs, outs):
            v2 = vec_d.rearrange("(c p) -> c p", p=128)
            done = 0
            while done < ncols:
                n = min(128, ncols - done)
                rb = nr % 4
                nr += 1
                tk = 'lc_r%d' % rb
                P.dma('sp', rows[rb][0:n, :], v2[done:done + n, :], w=[tk])
                P.pe(lambda e, rb=rb, n=n: e.transpose(ps[:, 0:n], rows[rb][0:n, :], C.idf[0:n, 0:n]),
                     r=[tk, 'idf'], w=['lc_ps'])
                P.dve(lambda e, n=n, d0=done, outt=outt: e.tensor_copy(outt[:, d0:d0 + n], ps[:, 0:n]),
                      w=['lc_ps'], wa=[name])
                done += n
        P.flush()
    return outs


def stage_ab(C):
    nc, P, I, Dm = C.nc, C.P, C.I, C.D
    with ExitStack() as st:
        hT = C.sbt(st, 'hT_ab', [128, 8, S], BF16)
        lc = load_cols_multi(C, st, [(I['b_branch_gate'], 16, 'bgate')] + [(I['conv_w'][k], 24, 'convw%d' % k) for k in range(4)]
                             + [(I['conv_b'], 24, 'convb')])
        bgate, convw, convb = lc[0], lc[1:5], lc[5]
        with ExitStack() as sa:
            g_bc = C.sbt(sa, 'a_g', [128, D], F32)
            b_bc = C.sbt(sa, 'a_b', [128, D], F32)
            load_ln_params(C, I['ln_in_g'], I['ln_in_b'], g_bc, b_bc)
            xin = [C.sbt(sa, 'a_x%d' % i, [128, D], F32) for i in range(2)]
            hh = [C.sbt(sa, 'a_h%d' % i, [128, D], F32) for i in range(2)]
            hb = [C.sbt(sa, 'a_hb%d' % i, [128, D], BF16) for i in range(2)]
            stt = [C.sbt(sa, 'a_st%d' % i, [128, 8], F32) for i in range(2)]
            junk = C.sbt(sa, 'a_junk', [128, D], BF16)
            ptr = [C.pst(sa, 'a_ptr%d' % i, [128, 8, 128], BF16) for i in range(2)]
            def a_p1(i):
                b = i % 2
                P.dma('sp', xin[b][:], I['x'][i * 128:(i + 1) * 128, :], w=['xin%d' % b])
                ln_stats(C, xin[b][:], 'xin%d' % b, stt[b], junk[:], 'a%d' % b)

            a_p1(0)
            for i in range(NT):
                b = i % 2
                if i + 1 < NT:
                    a_p1(i + 1)
                ln_apply(C, xin[b][:], 'xin%d' % b, hh[b][:], 'hh%d' % b, g_bc[:], b_bc[:], stt[b], 'a%d' % b)
                P.dma('pool', Dm['h0'][i * 128:(i + 1) * 128, :], hh[b][:], r=['hh%d' % b])
                P.act(lambda e, b=b: e.copy(hb[b][:], hh[b][:]), r=['hh%d' % b], w=['hb%d' % b])
                for c in range(8):
                    P.pe(lambda e, b=b, c=c: e.transpose(ptr[b][:, c, :], hb[b][:, c * 128:(c + 1) * 128], C.ident),
                         r=['hb%d' % b], w=['ptr%d' % b])
                P.dve(lambda e, b=b, i=i: e.tensor_copy(hT[:, :, i * 128:(i + 1) * 128], ptr[b][:]),
                      w=['ptr%d' % b, 'hT'])
            P.flush()
        with ExitStack() as sb:
            wblk = [C.sbt(sb, 'b_w%d' % i, [128, 8, 512], BF16) for i in range(2)]
            ps = [C.pst(sb, 'b_ps%d' % i, [128, 512], F32) for i in range(4)]
            stg = [C.sbt(sb, 'b_stg%d' % i, [128, S], BF16) for i in range(2)]
            nblk = [0]
            npsum = [0]
            nstg = [0]

            wlist = ([(c, 512) for c in (0, 512, 1024, 1536)] + [(8224 + i * 512, 512) for i in range(4)]
                     + [(2048, 512), (2560, 512)] + [(3072 + i * 512, 512) for i in range(4)] + [(8192, 32)]
                     + [(5120 + i * 512, 512) for i in range(6)])
            wissued = [0]

            def issue_w(n):
                c0, ncols = wlist[n]
                b = n % 2
                P.dma('pool', wblk[b][:, :, 0:ncols],
                      I['w_in'][:, c0:c0 + ncols].rearrange("(k p) c -> p k c", p=128), w=['wblk%d' % b])

            def load_w(c0, ncols):
                n = nblk[0]
                nblk[0] += 1
                assert wlist[n] == (c0, ncols), (n, wlist[n], c0, ncols)
                while wissued[0] <= min(n + 1, len(wlist) - 1):
                    issue_w(wissued[0])
                    wissued[0] += 1
                return n % 2

            def mm_fm(wb, cc, tg):
                pb = npsum[0] % 4
                npsum[0] += 1
                for k in range(8):
                    P.pe(lambda e, k=k, pb=pb: e.matmul(ps[pb][:], wblk[wb][:, k, cc * 128:(cc + 1) * 128],
                                                        hT[:, k, tg * 512:(tg + 1) * 512], start=(k == 0), stop=(k == 7)),
                         r=['wblk%d' % wb, 'hT'], w=['bps%d' % pb])
                return pb

            def fm_group(c0, ncols_total, dst, evac):
                for blk in range(ncols_total // 512):
                    wb = load_w(c0 + blk * 512, 512)
                    for cc in range(4):
                        sgb = nstg[0] % 2
                        nstg[0] += 1
                        chunk = blk * 4 + cc
                        for tg in range(8):
                            pb = mm_fm(wb, cc, tg)
                            evac(pb, stg[sgb][:, tg * 512:(tg + 1) * 512], chunk, 'stg%d' % sgb, tg)
                        P.dma('sp', dst[chunk * 128:(chunk + 1) * 128, :], stg[sgb][:], r=['stg%d' % sgb])

            def evac_copy(pb, dst, chunk, tok, tg):
                if tg % 2 == 0:
                    P.act(lambda e: e.copy(dst, ps[pb][:]), w=['bps%d' % pb], wa=[tok])
                else:
                    P.dve(lambda e: e.tensor_copy(dst, ps[pb][:]), w=['bps%d' % pb], wa=[tok])

            def evac_gate(pb, dst, chunk, tok, tg):
                P.act(lambda e: e.activation(dst, ps[pb][:], AF.Sigmoid, bias=bgate[:, chunk:chunk + 1]),
                      r=['bgate'], w=['bps%d' % pb], wa=[tok])

            fm_group(0, 1024, Dm['qT'], evac_copy)
            fm_group(1024, 1024, Dm['kT'], evac_copy)
            fm_group(8224, 2048, Dm['gT'], evac_gate)

            tstg = [C.sbt(sb, 'b_tstg%d' % i, [128, 4, 512], BF16) for i in range(2)]
            ntst = [0]

            def tm_group(c0, ncols_total, dst):
                for blk in range(ncols_total // 512):
                    wb = load_w(c0 + blk * 512, 512)
                    for t4 in range(NT // 4):
                        tb = ntst[0] % 2
                        ntst[0] += 1
                        for tt in range(4):
                            ti = t4 * 4 + tt
                            pb = npsum[0] % 4
                            npsum[0] += 1
                            for k in range(8):
                                P.pe(lambda e, k=k, pb=pb, ti=ti, wb=wb: e.matmul(ps[pb][:], hT[:, k, ti * 128:(ti + 1) * 128],
                                                                                  wblk[wb][:, k, :], start=(k == 0), stop=(k == 7)),
                                     r=['wblk%d' % wb, 'hT'], w=['bps%d' % pb])
                            if tt % 2 == 0:
                                P.act(lambda e, pb=pb, tb=tb, tt=tt: e.copy(tstg[tb][:, tt, :], ps[pb][:]),
                                      w=['bps%d' % pb], wa=['tstg%d' % tb])
                            else:
                                P.dve(lambda e, pb=pb, tb=tb, tt=tt: e.tensor_copy(tstg[tb][:, tt, :], ps[pb][:]),
                                      w=['bps%d' % pb], wa=['tstg%d' % tb])
                        P.dma('sp', dst[t4 * 512:(t4 + 1) * 512, blk * 512:(blk + 1) * 512].rearrange("(t p) c -> p t c", p=128),
                              tstg[tb][:], r=['tstg%d' % tb])

            tm_group(2048, 1024, Dm['v'])
            tm_group(3072, 2048, Dm['z'])

            dtb = C.sbt(sb, 'b_dtb', [128, 32], F32)
            abc = C.sbt(sb, 'b_abc', [128, 32], F32)
            dtt = [C.sbt(sb, 'b_dtt%d' % i, [128, 32], F32) for i in range(2)]
            P.dma('sp', dtb[:], I['dt_bias'].partition_broadcast(128), w=['dtb'])
            P.dma('sp', abc[:], I['a_log'].partition_broadcast(128), w=['abc'])
            P.act(lambda e: e.activation(abc[:], abc[:], AF.Exp), r=['abc'], w=['abc'])
            P.dve(lambda e: e.tensor_scalar(abc[:], abc[:], -1.0, None, ALU.mult), r=['abc'], w=['abc'])
            wb = load_w(8192, 32)
            for ti in range(NT):
                pb = npsum[0] % 4
                npsum[0] += 1
                b = ti % 2
                for k in range(8):
                    P.pe(lambda e, k=k, pb=pb, ti=ti, wb=wb: e.matmul(ps[pb][:, 0:32], hT[:, k, ti * 128:(ti + 1) * 128],
                                                                      wblk[wb][:, k, 0:32], start=(k == 0), stop=(k == 7)),
                         r=['wblk%d' % wb, 'hT'], w=['bps%d' % pb])
                P.dve(lambda e, pb=pb, b=b: e.tensor_tensor(dtt[b][:], ps[pb][:, 0:32], dtb[:], ALU.add),
                      r=['dtb'], w=['bps%d' % pb, 'dtt%d' % b])
                P.act(lambda e, b=b: e.activation(dtt[b][:], dtt[b][:], AF.Exp), r=['dtt%d' % b], w=['dtt%d' % b])
                P.act(lambda e, b=b, ti=ti: e.activation(C.dt_sb[:, ti, :], dtt[b][:], AF.Ln, bias=1.0),
                      r=['dtt%d' % b], w=['dt_sb'])
                P.dve(lambda e, ti=ti: e.tensor_tensor(C.da_bf[:, ti, :], C.dt_sb[:, ti, :], abc[:], ALU.mult),
                      r=['dt_sb', 'abc'], w=['da_bf'])

            U = [C.sbt(sb, 'b_U%d' % i, [128, S + 3], BF16) for i in range(2)]
            dg = C.sbt(sb, 'b_dg', [128, 96, 128], BF16)
            tps = [C.pst(sb, 'b_tps%d' % i, [128, 8, 128], BF16) for i in range(2)]
            cvp = [C.pst(sb, 'b_cvp%d' % i, [128, 512], F32) for i in range(2)]
            xtm = [C.sbt(sb, 'b_xtm%d' % i, [128, NT, 128], BF16) for i in range(2)]
            for i in range(2):
                P.dve(lambda e, i=i: e.memset(U[i][:, 0:3], 0.0), w=['U%d_m' % i])
            for chunk in range(24):
                for k in range(4):
                    P.dve(lambda e, chunk=chunk, k=k: e.tensor_scalar(dg[:, chunk * 4 + k, :], C.ident, convw[k][:, chunk:chunk + 1], None, ALU.mult),
                          r=['cst', 'convw%d' % k], wa=['dg'])
            ntp = [0]
            ncv = [0]
            for blk in range(6):
                wb = load_w(5120 + blk * 512, 512)
                for cc in range(4):
                    chunk = blk * 4 + cc
                    ub = chunk % 2

                    def conv_tg(tg, chunk=chunk, ub=ub):
                        cb = ncv[0] % 2
                        ncv[0] += 1
                        rt = ['U%d_%d' % (ub, tg), 'dg'] + (['U%d_%d' % (ub, tg - 1)] if tg > 0 else ['U%d_m' % ub])
                        for k in range(4):
                            P.pe(lambda e, k=k, cb=cb: e.matmul(cvp[cb][:], dg[:, chunk * 4 + k, :], U[ub][:, tg * 512 + k:tg * 512 + k + 512],
                                                                start=(k == 0), stop=(k == 3)), r=rt, w=['cvp%d' % cb])
                        P.act(lambda e, cb=cb: e.activation(stg[ub][:, tg * 512:(tg + 1) * 512], cvp[cb][:], AF.Silu, bias=convb[:, chunk:chunk + 1]),
                              r=['convb'], w=['cvp%d' % cb], wa=['stg%d' % ub])

                    for tg in range(8):
                        pb = mm_fm(wb, cc, tg)
                        if tg % 2 == 0:
                            P.act(lambda e, pb=pb, ub=ub, tg=tg: e.copy(U[ub][:, 3 + tg * 512:3 + (tg + 1) * 512], ps[pb][:]),
                                  w=['bps%d' % pb, 'U%d_%d' % (ub, tg)])
                        else:
                            P.dve(lambda e, pb=pb, ub=ub, tg=tg: e.tensor_copy(U[ub][:, 3 + tg * 512:3 + (tg + 1) * 512], ps[pb][:]),
                                  w=['bps%d' % pb, 'U%d_%d' % (ub, tg)])
                        if tg > 0:
                            conv_tg(tg - 1)
                    conv_tg(7)
                    if 16 <= chunk < 20:
                        P.dma('sp', Dm['bT'][(chunk - 16) * 128:(chunk - 15) * 128, :], stg[ub][:], r=['stg%d' % ub])
                    if chunk >= 20:
                        P.dma('sp', Dm['cT'][(chunk - 20) * 128:(chunk - 19) * 128, :], stg[ub][:], r=['stg%d' % ub])
                    if chunk < 20:
                        for t8 in range(4):
                            tb = ntp[0] % 2
                            ntp[0] += 1
                            for j in range(8):
                                ti = t8 * 8 + j
                                P.pe(lambda e, tb=tb, j=j, ti=ti, ub=ub: e.transpose(
                                    tps[tb][:, j, :], stg[ub][:, ti * 128:(ti + 1) * 128], C.ident),
                                    r=['stg%d' % ub], w=['tps%d' % tb])
                            if t8 % 2 == 0:
                                P.dve(lambda e, tb=tb, t8=t8, ub=ub: e.tensor_copy(xtm[ub][:, t8 * 8:(t8 + 1) * 8, :], tps[tb][:]),
                                      w=['tps%d' % tb], wa=['xtm%d' % ub])
                            else:
                                P.act(lambda e, tb=tb, t8=t8, ub=ub: e.copy(xtm[ub][:, t8 * 8:(t8 + 1) * 8, :], tps[tb][:]),
                                      w=['tps%d' % tb], wa=['xtm%d' % ub])
                        if chunk < 16:
                            dst = Dm['xs'][:, chunk * 128:(chunk + 1) * 128]
                        else:
                            dst = Dm['btm'][:, (chunk - 16) * 128:(chunk - 15) * 128]
                        P.dma('sp', dst.rearrange("(t p) c -> p t c", p=128), xtm[ub][:], r=['xtm%d' % ub])
            P.flush()


def stage_attn(C):
    nc, P, Dm = C.nc, C.P, C.D
    cs = C.cs
    with ExitStack() as st:
        V = C.sbt(st, 'c_V', [128, NT, D], BF16)
        QT = [C.sbt(st, 'c_QT%d' % i, [64, S], BF16) for i in range(2)]
        KT = [C.sbt(st, 'c_KT%d' % i, [64, S], BF16) for i in range(2)]
        osb = [C.sbt(st, 'c_osb%d' % i, [64, S], BF16) for i in range(2)]
        NB = 5
        E = [C.sbt(st, 'c_E%d' % i, [128, 512], F32) for i in range(NB)]
        SP = [C.sbt(st, 'c_SP%d' % i, [128, 512], BF16) for i in range(NB)]
        WT = [C.sbt(st, 'c_WT%d' % i, [128, 512], BF16) for i in range(NB)]
        LA = [C.sbt(st, 'c_LA%d' % i, [128, 512], BF16) for i in range(2)]
        zps = [C.pst(st, 'c_zps%d' % i, [128, 512], F32) for i in range(3)]
        cps = [C.pst(st, 'c_cps%d' % i, [128, 512], F32) for i in range(3)]
        ops_ = [C.pst(st, 'c_ops%d' % i, [128, 512], F32) for i in range(2)]
        for i in range(NB):
            P.dve(lambda e, i=i: e.memset(SP[i][:], 0.0), w=['SP%d' % i])
        vv = Dm['v'].rearrange("(t p) c -> p t c", p=128)
        I = C.I
        zt = C.sbt(st, 'zt', [128, 8 * 1032], BF16)
        P.pool(lambda e: e.memset(zt[:], 0.0), w=['zt'])
        zid = C.sbt(st, 'zid', [128, 8], mybir.dt.int32)
        P.dma('sp', zid[:], I['cst_i32'], w=['zid'])
        for j in range(8):
            P.pool(lambda e, j=j: e.tensor_copy(zt[:, j * 1032 + 1026:j * 1032 + 1028].bitcast(mybir.dt.int32), zid[:, j:j + 1]),
                   r=['zid'], w=['zt'])
        ztf = zt[:].bitcast(F32)
        zfills = [(C.xrows_d[i * 1024:(i + 1) * 1024, :].rearrange("(p j) c -> p (j c)", p=128), zt[:]) for i in range(32)]
        zfills += [(C.ff_d[i * 512:(i + 1) * 512, :].rearrange("(p j) c -> p (j c)", p=128), ztf[:, 0:4096]) for i in range(8)]
        tiles = []
        nqr = 0
        for h in range(16):
            for qr in range(8):
                nb = 4 * (qr + 1)
                for bi, kb in enumerate(range(nb - 1, -1, -1)):
                    tiles.append(dict(h=h, hb=h % 2, qr=qr, q0=qr * 512, bi=bi, kb=kb, nb=nb, ob=nqr % 2,
                                      diag=(kb >= 4 * qr), off=kb * 128 - qr * 512, lo=max(0, kb * 128 - qr * 512), last=(bi == nb - 1), i=len(tiles)))
                nqr += 1

        def load_head(h):
            hb = h % 2
            P.dma('sp', QT[hb][:], Dm['qT'][h * 64:(h + 1) * 64, :], w=['QT%d' % hb])
            P.dma('sp', KT[hb][:], Dm['kT'][h * 64:(h + 1) * 64, :], w=['KT%d' % hb])
            P.dve(lambda e, hb=hb: e.tensor_scalar(KT[hb][:], KT[hb][:], 0.125, None, ALU.mult),
                  r=['KT%d' % hb], w=['KT%d' % hb])

        def stage_a(t):
            i = t['i']
            zb = i % 3
            b4 = i % NB
            hb = t['hb']
            if t['h'] == 0 and t['qr'] == 0 and t['bi'] == 0:
                load_head(0)
                for q4 in range(4):
                    P.dma('sp', V[:, q4 * 8:(q4 + 1) * 8, :], vv[:, q4 * 8:(q4 + 1) * 8, :], wa=['V'])
            if t['qr'] == 1 and t['bi'] == 0 and t['h'] + 1 < 16:
                load_head(t['h'] + 1)
            lo = t['lo']
            ks = KT[hb][:, t['kb'] * 128:(t['kb'] + 1) * 128]
            qs = QT[hb][:, t['q0'] + lo:t['q0'] + 512]
            P.pe(lambda e: e.matmul(zps[zb][:, lo:512], ks, qs, start=True, stop=True), r=['QT%d' % hb, 'KT%d' % hb], w=['zps%d' % zb])
            P.act(lambda e: e.activation(E[b4][:, lo:512], zps[zb][:, lo:512], AF.Exp), w=['zps%d' % zb, 'E%d' % b4])

        def stage_a2(t):
            i = t['i']
            b4 = i % NB
            lo = t['lo']
            P.act(lambda e: e.activation(SP[b4][:, lo:512], E[b4][:, lo:512], AF.Ln, bias=1.0), r=['E%d' % b4], w=['SP%d' % b4])
            if t['diag']:
                off = t['off']
                if lo > 0:
                    P.dve(lambda e: e.memset(WT[b4][:, 0:lo], 0.0), w=['WT%d' % b4])
                P.dve(lambda e: e.tensor_tensor(SP[b4][:], SP[b4][:], cs('mask01', 384 - off, 512), ALU.mult),
                      r=['SP%d' % b4, 'cst'], w=['SP%d' % b4])

        def stage_b(t):
            i = t['i']
            cb = i % 3
            b4 = i % NB
            hb = t['hb']
            ob = t['ob']
            lo = t['lo']
            ks = KT[hb][:, t['kb'] * 128:(t['kb'] + 1) * 128]
            qs = QT[hb][:, t['q0'] + lo:t['q0'] + 512]
            mms = [(ks, qs, ['QT%d' % hb, 'KT%d' % hb]), (cs('negtri'), SP[b4][:, lo:512], ['cst', 'SP%d' % b4])]
            if t['bi'] > 0:
                mms.append((cs('negones'), LA[ob][:, lo:512], ['cst', 'LA%d' % ob]))
            if t['diag']:
                mms.append((C.ident, cs('negbig', 384 - t['off'] + lo, 512 - lo), ['cst']))
            for j, (l, r_, tk) in enumerate(mms):
                P.pe(lambda e, l=l, r_=r_, j=j, n=len(mms): e.matmul(cps[cb][:, lo:512], l, r_, start=(j == 0), stop=(j == n - 1)),
                     r=tk, w=['cps%d' % cb])
            P.act(lambda e: e.activation(WT[b4][:, lo:512], cps[cb][:, lo:512], AF.Exp), w=['cps%d' % cb], wa=['WT%d' % b4])
            if not t['last']:
                if t['bi'] == 0:
                    P.dve(lambda e: e.tensor_copy(LA[ob][:], SP[b4][:]), r=['SP%d' % b4], w=['LA%d' % ob])
                else:
                    P.dve(lambda e: e.tensor_tensor(LA[ob][:], LA[ob][:], SP[b4][:], ALU.add),
                           r=['SP%d' % b4, 'LA%d' % ob], w=['LA%d' % ob])

        def stage_c(t):
            i = t['i']
            b4 = i % NB
            hb = t['hb']
            ob = t['ob']
            h = t['h']
            kb = t['kb']
            q0 = t['q0']
            P.pe(lambda e: e.matmul(ops_[ob][0:64, :], V[:, kb, h * 64:(h + 1) * 64], WT[b4][:], start=(t['bi'] == 0), stop=t['last']),
                 r=['V', 'WT%d' % b4], w=['ops%d' % ob])
            if t['last']:
                P.dve(lambda e: e.tensor_copy(osb[hb][:, q0:q0 + 512], ops_[ob][0:64, :]), w=['ops%d' % ob, 'osb%d' % hb])
                if t['qr'] == 7:
                    P.dma('sp', Dm['osbT'][h * 64:(h + 1) * 64, :], osb[hb][:], r=['osb%d' % hb])

        n = len(tiles)
        for s_ in range(n + 3):
            if s_ == 4:
                for zo, zi in zfills:
                    P.dma('sp', zo, zi, r=['zt'])
            if s_ < n:
                stage_a(tiles[s_])
            if 0 <= s_ - 1 < n:
                stage_a2(tiles[s_ - 1])
            if 0 <= s_ - 2 < n:
                stage_b(tiles[s_ - 2])
            if 0 <= s_ - 3 < n:
                stage_c(tiles[s_ - 3])
        P.flush()


def stage_ssd(C):
    nc, P, I, Dm = C.nc, C.P, C.I, C.D
    cs = C.cs
    with ExitStack() as st:
        sbt = lambda n, s, d: C.sbt(st, n, s, d)
        pst = lambda n, s, d: C.pst(st, n, s, d)
        hst = sbt('d_hst', [128, 2048], F32)
        hbf = sbt('d_hbf', [128, 2048], BF16)
        dsk = sbt('d_dsk', [128, 32], F32)
        mtge = sbt('d_mtge', [128, 128], F32)
        xs = [sbt('d_xs%d' % i, [128, 2048], BF16) for i in range(2)]
        zz = [sbt('d_zz%d' % i, [128, 2048], BF16) for i in range(2)]
        btm = [sbt('d_btm%d' % i, [128, 512], BF16) for i in range(2)]
        bT = [sbt('d_bT%d' % i, [128, 4, 128], BF16) for i in range(2)]
        cT = [sbt('d_cT%d' % i, [128, 4, 128], BF16) for i in range(2)]
        ex3 = [sbt('d_ex3%d' % i, [128, 96], F32) for i in range(2)]
        xdt = [sbt('d_xdt%d' % i, [128, 2048], BF16) for i in range(2)]
        xw = [sbt('d_xw%d' % i, [128, 2048], BF16) for i in range(2)]
        rbig = [sbt('d_rbig%d' % i, [128, 4096], BF16) for i in range(2)]
        seg = [sbt('d_seg%d' % i, [128, 1024], BF16) for i in range(2)]
        GT4 = [sbt('d_GT%d' % i, [128, 4, 1024], BF16) for i in range(2)]
        cbm = [sbt('d_cbm%d' % i, [128, 128], BF16) for i in range(2)]
        t1 = [sbt('d_t1%d' % i, [128, 512], F32) for i in range(2)]
        t2 = [sbt('d_t2%d' % i, [128, 512], F32) for i in range(2)]
        ysb = [sbt('d_y%d' % i, [128, 2048], F32) for i in range(2)]
        dskI = sbt('d_dskI', [128, 32, 128], BF16)
        sz = [sbt('d_sz%d' % i, [128, 2048], F32) for i in range(2)]
        sst = sbt('d_sst', [128, 8], F32)
        jnk = sbt('d_jnk', [128, 512], BF16)
        ob = [sbt('d_ob%d' % i, [128, 2048], BF16) for i in range(2)]
        sm_ps = pst('d_smps', [128, 512], F32)
        S_ps = pst('d_Sps', [128, 1024], F32)
        cb_ps = pst('d_cbps', [128, 512], F32)
        y_ps = pst('d_yps', [128, 512], F32)
        yi_ps = pst('d_yips', [128, 512], F32)
        hn_ps = pst('d_hnps', [128, 512], F32)

        P.dma('sp', dsk[:], I['d_skip'].partition_broadcast(128), w=['dsk'])
        P.dve(lambda e: e.memset(hst[:], 0.0), w=['hst%d' % g for g in range(4)])
        P.dve(lambda e: e.memset(hbf[:], 0.0), w=['hbf%d' % g for g in range(4)])
        P.dve(lambda e: e.tensor_copy(mtge[:], cs('mle')), r=['cst'], w=['mtge'])
        for h in range(32):
            P.dve(lambda e, h=h: e.tensor_scalar(dskI[:, h, :], C.ident, dsk[:, h:h + 1], None, ALU.mult), r=['cst', 'dsk'], wa=['dskI'])
        bTv = Dm['bT'].rearrange("(g n) t -> n g t", n=128)
        cTv = Dm['cT'].rearrange("(g n) t -> n g t", n=128)
        def stage1(c):
            b = c % 2
            r0 = c * 128
            P.dma('sp', xs[b][:], Dm['xs'][r0:r0 + 128, :], w=['xs%d' % b])
            P.dma('sp', zz[b][:], Dm['z'][r0:r0 + 128, :], w=['zz%d' % b])
            P.dma('sp', btm[b][:], Dm['btm'][r0:r0 + 128, :], w=['btm%d' % b])
            P.dma('sp', bT[b][:], bTv[:, :, r0:r0 + 128], w=['bT%d' % b])
            P.dma('sp', cT[b][:], cTv[:, :, r0:r0 + 128], w=['cT%d' % b])
            da_c = C.da_bf[:, c, :]
            dt_c = C.dt_sb[:, c, :]
            P.pe(lambda e: e.matmul(sm_ps[:, 0:32], cs('mle'), da_c, start=True, stop=True), r=['cst'], w=['smps'])
            P.pe(lambda e: e.matmul(sm_ps[:, 32:64], cs('mgt'), da_c, start=True, stop=True), r=['cst'], w=['smps'])
            P.pe(lambda e: e.matmul(sm_ps[:, 64:96], cs('ones'), da_c, start=True, stop=True), r=['cst'], w=['smps'])
            P.act(lambda e: e.activation(ex3[b][:], sm_ps[:, 0:96], AF.Exp), w=['smps', 'ex3%d' % b])
            P.dve(lambda e: e.tensor_tensor(
                xdt[b][:].rearrange("p (h c) -> p h c", h=32), xs[b][:].rearrange("p (h c) -> p h c", h=32),
                dt_c.unsqueeze(2).to_broadcast([128, 32, 64]), ALU.mult), r=['xs%d' % b], w=['xdt%d' % b])
            P.pool(lambda e: e.tensor_tensor(
                xw[b][:].rearrange("p (h c) -> p h c", h=32), xdt[b][:].rearrange("p (h c) -> p h c", h=32),
                ex3[b][:, 32:64].unsqueeze(2).to_broadcast([128, 32, 64]), ALU.mult),
                r=['xdt%d' % b, 'ex3%d' % b], w=['xw%d' % b])
            P.dve(lambda e: e.tensor_tensor(
                rbig[b][:].rearrange("p (h t) -> p h t", h=32), cs('mle').unsqueeze(1).to_broadcast([128, 32, 128]),
                da_c.unsqueeze(2).to_broadcast([128, 32, 128]), ALU.mult), r=['cst'], w=['rbig%d' % b])
            P.act(lambda e: e.activation(sz[b][:], zz[b][:], AF.Silu), r=['zz%d' % b], w=['sz%d' % b])
            for g in range(4):
                gb = (c * 4 + g) % 2
                for hf in range(2):
                    P.pe(lambda e, g=g, hf=hf: e.matmul(S_ps[:, hf * 512:(hf + 1) * 512], cs('mgt'),
                                                        rbig[b][:, g * 1024 + hf * 512:g * 1024 + (hf + 1) * 512],
                                                        start=True, stop=True), r=['cst', 'rbig%d' % b], w=['Sps'])
                P.act(lambda e, gb=gb: e.activation(seg[gb][:], S_ps[:], AF.Exp), w=['Sps', 'seg%d' % gb])
                P.pe(lambda e, g=g: e.matmul(cb_ps[:, 0:128], bT[b][:, g, :], cT[b][:, g, :], start=True, stop=True),
                     r=['bT%d' % b, 'cT%d' % b], w=['cbps'])
                P.dve(lambda e, gb=gb: e.tensor_tensor(cbm[gb][:], cb_ps[:, 0:128], mtge[:], ALU.mult), r=['mtge'], w=['cbps', 'cbm%d' % gb])
                eng = P.pool
                eng(lambda e, gb=gb, g=g: e.tensor_tensor(
                    GT4[b][:, g, :].rearrange("p (r t) -> p r t", r=8), seg[gb][:].rearrange("p (r t) -> p r t", r=8),
                    cbm[gb][:].unsqueeze(1).to_broadcast([128, 8, 128]), ALU.mult),
                    r=['seg%d' % gb, 'cbm%d' % gb], w=['GT%d%d' % (b, g)])

        def stage2(c):
            b = c % 2
            r0 = c * 128
            for g in range(4):
                gb = (c * 4 + g) % 2
                for r in range(8):
                    hh = g * 8 + r
                    P.pe(lambda e, g=g, r=r, hh=hh: e.matmul(
                        y_ps[:, r * 64:(r + 1) * 64], GT4[b][:, g, r * 128:(r + 1) * 128], xdt[b][:, hh * 64:(hh + 1) * 64],
                        start=True, stop=False), r=['GT%d%d' % (b, g), 'xdt%d' % b], w=['yps'])
                    P.pe(lambda e, r=r, hh=hh: e.matmul(
                        y_ps[:, r * 64:(r + 1) * 64], dskI[:, hh, :], xs[b][:, hh * 64:(hh + 1) * 64],
                        start=False, stop=True), r=['dskI', 'xs%d' % b], w=['yps'])
                P.pe(lambda e, g=g: e.matmul(yi_ps[:], cT[b][:, g, :], hbf[:, g * 512:(g + 1) * 512], start=True, stop=True),
                     r=['cT%d' % b, 'hbf%d' % g], w=['yips'])
                P.dve(lambda e, gb=gb, g=g: e.tensor_tensor(
                    t1[gb][:].rearrange("p (r c) -> p r c", r=8), yi_ps[:].rearrange("p (r c) -> p r c", r=8),
                    ex3[b][:, g * 8:(g + 1) * 8].unsqueeze(2).to_broadcast([128, 8, 64]), ALU.mult),
                    r=['ex3%d' % b], w=['yips', 't1%d' % gb])
                P.dve(lambda e, gb=gb, g=g: e.tensor_tensor(ysb[b][:, g * 512:(g + 1) * 512], t1[gb][:], y_ps[:], ALU.add),
                      r=['t1%d' % gb], w=['yps', 'ysb%d%d' % (b, g)])
                P.pe(lambda e, g=g: e.matmul(hn_ps[:], btm[b][:, g * 128:(g + 1) * 128], xw[b][:, g * 512:(g + 1) * 512],
                                             start=True, stop=True), r=['btm%d' % b, 'xw%d' % b], w=['hnps'])
                P.dve(lambda e, gb=gb, g=g: e.tensor_tensor(
                    t2[gb][:].rearrange("p (r c) -> p r c", r=8), hst[:, g * 512:(g + 1) * 512].rearrange("p (r c) -> p r c", r=8),
                    ex3[b][:, 64 + g * 8:64 + (g + 1) * 8].unsqueeze(2).to_broadcast([128, 8, 64]), ALU.mult),
                    r=['hst%d' % g, 'ex3%d' % b], w=['t2%d' % gb])
                P.dve(lambda e, gb=gb, g=g: e.tensor_tensor(hst[:, g * 512:(g + 1) * 512], t2[gb][:], hn_ps[:], ALU.add),
                      r=['t2%d' % gb], w=['hnps', 'hst%d' % g])
                P.act(lambda e, g=g: e.copy(hbf[:, g * 512:(g + 1) * 512], hst[:, g * 512:(g + 1) * 512]),
                      r=['hst%d' % g], w=['hbf%d' % g])
            ytoks = ['ysb%d%d' % (b, g) for g in range(4)]
            P.dve(lambda e: e.tensor_tensor(ysb[b][:], ysb[b][:], sz[b][:], ALU.mult), r=ytoks + ['sz%d' % b], w=ytoks)
            P.pool(lambda e: e.memset(sst[:, 0:4], 0.0), w=['ss%d' % g for g in range(4)])
            for g in range(4):
                P.act(lambda e, g=g: e.activation(jnk[:], ysb[b][:, g * 512:(g + 1) * 512], AF.Square,
                                                  accum_out=sst[:, g:g + 1]), r=ytoks, w=['jnk', 'ss%d' % g])
            sstk = ['ss%d' % g for g in range(4)]
            P.dve(lambda e: e.tensor_scalar(sst[:, 0:4], sst[:, 0:4], 1.0 / 512, EPS, ALU.mult, ALU.add), r=sstk, w=sstk)
            P.act(lambda e: e.activation(sst[:, 0:4], sst[:, 0:4], AF.Ln), r=sstk, w=sstk)
            P.act(lambda e: e.activation(sst[:, 4:8], sst[:, 0:4], AF.Exp, scale=-0.5), r=sstk, w=['rstd4'])
            P.dve(lambda e: e.tensor_tensor(
                ob[b][:].rearrange("p (g c) -> p g c", g=4), ysb[b][:].rearrange("p (g c) -> p g c", g=4),
                sst[:, 4:8].unsqueeze(2).to_broadcast([128, 4, 512]), ALU.mult), r=ytoks + ['rstd4'], w=['ob%d' % b])
            P.dma('sp', Dm['ossd'][r0:r0 + 128, :], ob[b][:], r=['ob%d' % b])

        stage1(0)
        for c in range(NT):
            if c + 1 < NT:
                stage1(c + 1)
            stage2(c)
        P.flush()


def load_weight(C, st, name, w_d, kchunks, ncols, stg=None, defer=False):
    P = C.P
    t = C.sbt(st, name, [128, kchunks, ncols], BF16)
    if defer:
        return t, (lambda: _load_weight_into(C, t, name, w_d, kchunks, stg))
    _load_weight_into(C, t, name, w_d, kchunks, stg)
    return t


def _load_weight_into(C, t, name, w_d, kchunks, stg):
    P = C.P
    wv = w_d.rearrange("(k p) c -> p k c", p=128)
    if stg is None:
        for k0 in range(0, kchunks, 4):
            P.dma('pool', t[:, k0:k0 + 4, :], wv[:, k0:k0 + 4, :], wa=[name])
        return
    for k0 in range(0, kchunks, 2):
        j = C.nstg % len(stg)
        C.nstg += 1
        P.dma('sp', stg[j][:], wv[:, k0:k0 + 2, :], w=['wstg%d' % j])
        dst = t[:, k0:k0 + 2, :]
        if C.nstg % 2 == 0:
            P.act(lambda e, j=j, dst=dst: e.copy(dst, stg[j][:]), r=['wstg%d' % j], wa=[name])
        else:
            P.dve(lambda e, j=j, dst=dst: e.tensor_copy(dst, stg[j][:]), r=['wstg%d' % j], wa=[name])


def stage_merge(C):
    nc, P, I, Dm = C.nc, C.P, C.I, C.D
    with ExitStack() as st:
        ngc = C.ngc
        sbt = lambda n, s, d: C.sbt(st, n, s, d)
        pst = lambda n, s, d: C.pst(st, n, s, d)
        wst = [sbt('e_wst%d' % i, [128, 2, D], F32) for i in range(2)]
        w_sb = load_weight(C, st, 'e_wsb', I['w_sb'], 8, D, wst)
        w_ssd = load_weight(C, st, 'e_wssd', I['w_ssd'], 16, D, wst)
        for k in range(16):
            P.dve(lambda e, k=k: e.tensor_scalar(w_ssd[:, k, :], w_ssd[:, k, :], ngc[:, k:k + 1], None, ALU.mult),
                  r=['e_wssd', 'e_ngc'], w=['e_wssd'])
        w_mix = load_weight(C, st, 'e_wmix', I['w_mix_out'], 8, D, wst)
        g_bc = sbt('e_g', [128, D], F32)
        b_bc = sbt('e_b', [128, D], F32)
        load_ln_params(C, I['ln1_g'], I['ln1_b'], g_bc, b_bc)
        osbT = sbt('e_osbT', [128, 8, 512], BF16)
        gT = sbt('e_gT', [128, 16, 512], BF16)
        ossd = sbt('e_ossd', [128, 4, 2048], BF16)
        ossdT = sbt('e_ossdT', [128, 16, 512], BF16)
        h0t = [sbt('e_h0%d' % i, [128, D], F32) for i in range(2)]
        mT = sbt('e_mT', [128, 8, 512], BF16)
        ta = [sbt('e_ta%d' % i, [128, 512], F32) for i in range(2)]
        tb = [sbt('e_tb%d' % i, [128, 512], F32) for i in range(2)]
        res = [sbt('e_res%d' % i, [128, D], F32) for i in range(2)]
        h1t = [sbt('e_h1%d' % i, [128, D], F32) for i in range(2)]
        stt = [sbt('e_st%d' % i, [128, 8], F32) for i in range(2)]
        junk = sbt('e_junk', [128, D], BF16)
        tp = [pst('e_tp%d' % i, [128, 8, 128], BF16) for i in range(2)]
        A_ps = [pst('e_A%d' % i, [128, 512], F32) for i in range(2)]
        B_ps = [pst('e_B%d' % i, [128, 512], F32) for i in range(2)]
        mix_ps = pst('e_mix', [128, D], F32)
        osbv = Dm['osbT'].rearrange("(k p) t -> p k t", p=128)
        gTv = Dm['gT'].rearrange("(k p) t -> p k t", p=128)
        ntp = 0
        nn = 0
        nres = 0
        def e_loads(tg):
            t0 = tg * 512
            P.dma('sp', osbT[:], osbv[:, :, t0:t0 + 512], w=['osbT'])
            P.dma('sp', gT[:], gTv[:, :, t0:t0 + 512], w=['gT'])
            P.dma('sp', ossd[:], Dm['ossd'][t0:t0 + 512, :].rearrange("(t p) c -> p t c", p=128), w=['ossd'])

        e_loads(0)
        for tg in range(8):
            t0 = tg * 512
            for tt in range(4):
                for hf in range(2):
                    pb = ntp % 2
                    ntp += 1
                    for j in range(8):
                        ch = hf * 8 + j
                        P.pe(lambda e, pb=pb, j=j, tt=tt, ch=ch: e.transpose(tp[pb][:, j, :], ossd[:, tt, ch * 128:(ch + 1) * 128], C.ident),
                             r=['ossd'], w=['tp%d' % pb])
                    if pb == 0:
                        P.act(lambda e, pb=pb, hf=hf, tt=tt: e.copy(ossdT[:, hf * 8:(hf + 1) * 8, tt * 128:(tt + 1) * 128], tp[pb][:]),
                              w=['tp%d' % pb], wa=['ossdT'])
                    else:
                        P.dve(lambda e, pb=pb, hf=hf, tt=tt: e.tensor_copy(ossdT[:, hf * 8:(hf + 1) * 8, tt * 128:(tt + 1) * 128], tp[pb][:]),
                              w=['tp%d' % pb], wa=['ossdT'])
            for nck in range(8):
                pb = nn % 2
                nn += 1
                for k in range(8):
                    P.pe(lambda e, pb=pb, k=k, nck=nck: e.matmul(A_ps[pb][:], w_sb[:, k, nck * 128:(nck + 1) * 128], osbT[:, k, :],
                                                                 start=(k == 0), stop=(k == 7)), r=['e_wsb', 'osbT'], w=['A%d' % pb])
                for k in range(16):
                    P.pe(lambda e, pb=pb, k=k, nck=nck: e.matmul(B_ps[pb][:], w_ssd[:, k, nck * 128:(nck + 1) * 128], ossdT[:, k, :],
                                                                 start=(k == 0), stop=(k == 15)), r=['e_wssd', 'ossdT'], w=['B%d' % pb])
                P.dve(lambda e, pb=pb, nck=nck: e.tensor_tensor(ta[pb][:], A_ps[pb][:], gT[:, nck, :], ALU.mult),
                      r=['gT'], w=['A%d' % pb, 'ta%d' % pb])
                P.dve(lambda e, pb=pb, nck=nck: e.tensor_tensor(tb[pb][:], B_ps[pb][:], gT[:, 8 + nck, :], ALU.mult),
                      r=['gT'], w=['B%d' % pb, 'tb%d' % pb])
                P.pool(lambda e, pb=pb, nck=nck: e.tensor_tensor(mT[:, nck, :], ta[pb][:], tb[pb][:], ALU.add),
                       r=['ta%d' % pb, 'tb%d' % pb], w=['mT'])
            if tg + 1 < 8:
                e_loads(tg + 1)
            for tt in range(4):
                rb = nres % 2
                nres += 1
                r0 = t0 + tt * 128
                P.dma('act', h0t[rb][:], Dm['h0'][r0:r0 + 128, :], w=['h0t%d' % rb])
                for hf in range(2):
                    for k in range(8):
                        P.pe(lambda e, k=k, tt=tt, hf=hf: e.matmul(mix_ps[:, hf * 512:(hf + 1) * 512], mT[:, k, tt * 128:(tt + 1) * 128],
                                                                   w_mix[:, k, hf * 512:(hf + 1) * 512], start=(k == 0), stop=(k == 7)),
                             r=['mT', 'e_wmix'], w=['mix'])
                P.dve(lambda e, rb=rb, tt=tt: e.scalar_tensor_tensor(res[rb][:], h0t[rb][:], ALPHA, mix_ps[:], ALU.mult, ALU.add),
                      r=['h0t%d' % rb], w=['mix', 'res%d' % rb])
                ln_tile(C, res[rb][:], 'res%d' % rb, h1t[rb][:], 'h1t%d' % rb, g_bc[:], b_bc[:], stt[rb], junk[:], 'e%d' % rb)
                r0 = t0 + tt * 128
                P.dma('sp', Dm['h1'][r0:r0 + 128, :], h1t[rb][:], r=['h1t%d' % rb])
        P.flush()


def stage_xattn(C):
    nc, P, I, Dm = C.nc, C.P, C.I, C.D
    with ExitStack() as st:
        sbt = lambda n, s, d: C.sbt(st, n, s, d)
        pst = lambda n, s, d: C.pst(st, n, s, d)
        wst = [sbt('f_wst%d' % i, [128, 2, D], F32) for i in range(4)]
        w_xq, ld_xq = load_weight(C, st, 'f_wq', I['w_xq'], 8, D, wst, defer=True)
        w_xo, ld_xo = load_weight(C, st, 'f_wo', I['w_xo'], 8, D, wst, defer=True)
        KT = sbt('f_KT', [128, 8, MEM], BF16)
        Vm = sbt('f_V', [128, 2, D], BF16)
        g_bc = sbt('f_g', [128, D], F32)
        b_bc = sbt('f_b', [128, D], F32)
        load_ln_params(C, I['ln2_g'], I['ln2_b'], g_bc, b_bc)
        tpB = pst('f_tp', [128, 8, 128], BF16)
        q_ps = pst('f_qps', [128, 512], F32)
        s_ps = pst('f_sps', [128, 4, MEM], F32)
        OT_ps = pst('f_OTps', [128, 8, 128], F32)
        xa_ps = pst('f_xaps', [128, D], F32)
        with ExitStack() as pro:
            w_xk = load_weight(C, pro, 'f_wk', I['w_xk'], 8, D, wst)
            w_xv = load_weight(C, pro, 'f_wv', I['w_xv'], 8, D, wst)
            ld_xq()
            ld_xo()
            memf = C.sbt(pro, 'f_memf', [128, D], F32)
            memb = C.sbt(pro, 'f_memb', [128, D], BF16)
            memT = C.sbt(pro, 'f_memT', [128, 8, MEM], BF16)
            for mt in range(2):
                P.dma('sp', memf[:], I['mem'][mt * 128:(mt + 1) * 128, :], w=['memf'])
                P.act(lambda e: e.copy(memb[:], memf[:]), r=['memf'], w=['memb'])
                for c in range(8):
                    P.pe(lambda e, c=c: e.transpose(tpB[:, c, :], memb[:, c * 128:(c + 1) * 128], C.ident), r=['memb'], w=['tpB'])
                P.dve(lambda e, mt=mt: e.tensor_copy(memT[:, :, mt * 128:(mt + 1) * 128], tpB[:]), w=['tpB', 'memT'])
            for cc in range(8):
                for k in range(8):
                    P.pe(lambda e, cc=cc, k=k: e.matmul(q_ps[:, 0:MEM], w_xk[:, k, cc * 128:(cc + 1) * 128], memT[:, k, :],
                                                        start=(k == 0), stop=(k == 7)), r=['f_wk', 'memT'], w=['qps'])
                P.act(lambda e, cc=cc: e.copy(KT[:, cc, :], q_ps[:, 0:MEM]), w=['qps', 'KT'])
            for mt in range(2):
                for hf in range(2):
                    for k in range(8):
                        P.pe(lambda e, mt=mt, hf=hf, k=k: e.matmul(xa_ps[:, hf * 512:(hf + 1) * 512], memT[:, k, mt * 128:(mt + 1) * 128],
                                                                   w_xv[:, k, hf * 512:(hf + 1) * 512], start=(k == 0), stop=(k == 7)),
                             r=['f_wv', 'memT'], w=['xaps'])
                P.act(lambda e, mt=mt: e.copy(Vm[:, mt, :], xa_ps[:]), w=['xaps', 'Vm'])
            P.flush()
        h1f = [sbt('f_h1f%d' % i, [128, 4, D], F32) for i in range(2)]
        h1b = sbt('f_h1b', [128, D], BF16)
        h1T = [sbt('f_h1T%d' % i, [128, 8, 512], BF16) for i in range(2)]
        QT = [sbt('f_QT%d' % i, [128, 8, 512], BF16) for i in range(2)]
        pex = [sbt('f_pex%d' % i, [128, 4, MEM], F32) for i in range(2)]
        pn = [sbt('f_pn%d' % i, [128, 4 * MEM], BF16) for i in range(2)]
        PT = [sbt('f_PT%d' % i, [128, 8, 128], BF16) for i in range(2)]
        OT = sbt('f_OT', [128, 8, 128], BF16)
        sm = [sbt('f_sm%d' % i, [128, 16], F32) for i in range(2)]
        res = [sbt('f_res%d' % i, [128, D], F32) for i in range(2)]
        h2t = [sbt('f_h2%d' % i, [128, D], F32) for i in range(2)]
        stt = [sbt('f_st%d' % i, [128, 8], F32) for i in range(2)]
        junk = sbt('f_junk', [128, D], BF16)

        def prep(tg):
            gb = tg % 2
            t0 = tg * 512
            P.dma('sp', h1f[gb][:], Dm['h1'][t0:t0 + 512, :].rearrange("(t p) c -> p t c", p=128), w=['h1f%d' % gb])
            for tt in range(4):
                P.act(lambda e, tt=tt: e.copy(h1b[:], h1f[gb][:, tt, :]), r=['h1f%d' % gb], w=['h1b'])
                for c in range(8):
                    P.pe(lambda e, c=c: e.transpose(tpB[:, c, :], h1b[:, c * 128:(c + 1) * 128], C.ident), r=['h1b'], w=['tpB'])
                P.dve(lambda e, tt=tt: e.tensor_copy(h1T[gb][:, :, tt * 128:(tt + 1) * 128], tpB[:]), w=['tpB', 'h1T%d' % gb])
            for cc in range(8):
                for k in range(8):
                    P.pe(lambda e, cc=cc, k=k: e.matmul(q_ps[:], w_xq[:, k, cc * 128:(cc + 1) * 128], h1T[gb][:, k, :],
                                                        start=(k == 0), stop=(k == 7)), r=['f_wq', 'h1T%d' % gb], w=['qps'])
                P.act(lambda e, cc=cc: e.mul(QT[gb][:, cc, :], q_ps[:], 1.0 / 16.0), w=['qps', 'QT%d' % gb])

        def s1(ti):
            tg, tt = ti // 4, ti % 4
            gb = tg % 2
            rb = ti % 2
            ts = slice(tt * 128, (tt + 1) * 128)
            for h in range(4):
                for kk in range(2):
                    P.pe(lambda e, h=h, kk=kk: e.matmul(s_ps[:, h, :], QT[gb][:, 2 * h + kk, ts], KT[:, 2 * h + kk, :],
                                                        start=(kk == 0), stop=(kk == 1)), r=['QT%d' % gb, 'KT'], w=['sps'])
            smt = sm[rb]
            P.dve(lambda e: e.tensor_reduce(smt[:, 0:4], s_ps[:], axis=AX.X, op=ALU.max), w=['sps', 'mx%d' % rb])
            P.dve(lambda e: e.tensor_scalar(smt[:, 4:8], smt[:, 0:4], -1.0, None, ALU.mult), r=['mx%d' % rb], w=['nmx%d' % rb])
            P.pool(lambda e: e.memset(smt[:, 8:12], 0.0), w=['sum%d%d' % (rb, h) for h in range(4)])
            for h in range(4):
                P.act(lambda e, h=h: e.activation(pex[rb][:, h, :], s_ps[:, h, :], AF.Exp, bias=smt[:, 4 + h:5 + h],
                                                  accum_out=smt[:, 8 + h:9 + h]),
                      r=['nmx%d' % rb], w=['sps', 'pex%d' % rb, 'sum%d%d' % (rb, h)])
            sumt = ['sum%d%d' % (rb, h) for h in range(4)]
            P.dve(lambda e: e.reciprocal(smt[:, 12:16], smt[:, 8:12]), r=sumt, w=['rs%d' % rb])
            P.dve(lambda e: e.tensor_tensor(pn[rb][:].rearrange("p (h m) -> p h m", h=4), pex[rb][:],
                                            smt[:, 12:16].unsqueeze(2).to_broadcast([128, 4, MEM]), ALU.mult),
                  r=['pex%d' % rb, 'rs%d' % rb], w=['pn%d' % rb])
            for j in range(8):
                P.pe(lambda e, j=j: e.transpose(tpB[:, j, :], pn[rb][:, j * 128:(j + 1) * 128], C.ident), r=['pn%d' % rb], w=['tpB'])
            P.dve(lambda e: e.tensor_copy(PT[rb][:], tpB[:]), w=['tpB', 'PT%d' % rb])

        def s2(ti):
            tg, tt = ti // 4, ti % 4
            gb = tg % 2
            rb = ti % 2
            for cc in range(8):
                h = cc // 2
                for mt in range(2):
                    P.pe(lambda e, cc=cc, h=h, mt=mt: e.matmul(OT_ps[:, cc, :], Vm[:, mt, cc * 128:(cc + 1) * 128], PT[rb][:, 2 * h + mt, :],
                                                               start=(mt == 0), stop=(mt == 1)), r=['Vm', 'PT%d' % rb], w=['OTps'])
            P.act(lambda e: e.copy(OT[:], OT_ps[:]), w=['OTps', 'OT'])
            for hf in range(2):
                for cc in range(8):
                    P.pe(lambda e, hf=hf, cc=cc: e.matmul(xa_ps[:, hf * 512:(hf + 1) * 512], OT[:, cc, :], w_xo[:, cc, hf * 512:(hf + 1) * 512],
                                                          start=(cc == 0), stop=(cc == 7)), r=['OT', 'f_wo'], w=['xaps'])
            P.dve(lambda e: e.scalar_tensor_tensor(res[rb][:], h1f[gb][:, tt, :], ALPHA, xa_ps[:], ALU.mult, ALU.add),
                  r=['h1f%d' % gb], w=['xaps', 'res%d' % rb])
            ln_tile(C, res[rb][:], 'res%d' % rb, h2t[rb][:], 'h2t%d' % rb, g_bc[:], b_bc[:], stt[rb], junk[:], 'f%d' % rb)
            P.dma('sp', Dm['h2'][ti * 128:(ti + 1) * 128, :], h2t[rb][:], r=['h2t%d' % rb])

        prep(0)
        for ti in range(NT + 1):
            if ti < NT:
                s1(ti)
                if ti % 4 == 1 and ti // 4 + 1 < 8:
                    prep(ti // 4 + 1)
            if ti >= 1:
                s2(ti - 1)
        P.flush()


CAP = 1024
NSLOT = NE * CAP
XROW = 1032


def stage_moe(C):
    nc, P, I, Dm = C.nc, C.P, C.I, C.D
    xrows_d = C.xrows_d
    yrows_d = C.yrows_d
    with ExitStack() as st:
        bgc, buc = C.bgc, C.buc
        NW = 5
        W = [C.sbt(st, 'g_W%d' % i, [128, 8, D], BF16) for i in range(NW)]
        stg32 = [C.sbt(st, 'g_stg%d' % i, [128, 2, D], F32) for i in range(4)]
        desti = C.sbt(st, 'g_desti', [128, NT, 4], mybir.dt.int32)
        nw = [0]
        nst = [0]

        def loadW(src):
            i = nw[0] % NW
            nw[0] += 1
            wv = src.rearrange("(k p) c -> p k c", p=128)
            for k0 in (0, 2, 4, 6):
                j = nst[0] % 4
                nst[0] += 1
                P.dma('sp', stg32[j][:], wv[:, k0:k0 + 2, :], w=['stg32%d' % j])
                dst = W[i][:, k0:k0 + 2, :]
                if j == 0:
                    P.act(lambda e, j=j, dst=dst: e.copy(dst, stg32[j][:]), r=['stg32%d' % j], wa=['W%d' % i])
                elif j == 2:
                    P.act(lambda e, j=j, dst=dst: e.copy(dst, stg32[j][:]), r=['stg32%d' % j], wa=['W%d' % i])
                else:
                    P.dve(lambda e, j=j, dst=dst: e.tensor_copy(dst, stg32[j][:]), r=['stg32%d' % j], wa=['W%d' % i])
            return i

        wq = {'items': [], 'dma_i': 0, 'cast_i': 0}

        def queueW(src):
            i = nw[0] % NW
            nw[0] += 1
            wv = src.rearrange("(k p) c -> p k c", p=128)
            for k0 in (0, 2, 4, 6):
                wq['items'].append((wv[:, k0:k0 + 2, :], W[i][:, k0:k0 + 2, :], 'W%d' % i))
            return i

        def pumpW(n=1):
            for _ in range(n):
                while wq['dma_i'] < len(wq['items']) and wq['dma_i'] < wq['cast_i'] + 3:
                    srcq, _, _ = wq['items'][wq['dma_i']]
                    j = wq['dma_i'] % 4
                    P.dma('sp', stg32[j][:], srcq, w=['stg32%d' % j])
                    wq['dma_i'] += 1
                if wq['cast_i'] < wq['dma_i']:
                    _, dst, tok = wq['items'][wq['cast_i']]
                    j = wq['cast_i'] % 4
                    if j % 2 == 0:
                        P.act(lambda e, j=j, dst=dst: e.copy(dst, stg32[j][:]), r=['stg32%d' % j], wa=[tok])
                    else:
                        P.dve(lambda e, j=j, dst=dst: e.tensor_copy(dst, stg32[j][:]), r=['stg32%d' % j], wa=[tok])
                    wq['cast_i'] += 1

        with ExitStack() as ro:
            sbt = lambda n, s, d: C.sbt(ro, n, s, d)
            pst = lambda n, s, d: C.pst(ro, n, s, d)
            wr = sbt('r_w', [128, 8, NE], F32)
            brb = sbt('r_b', [128, NE], F32)
            P.dma('sp', wr[:], I['w_router'].rearrange("(k p) c -> p k c", p=128), w=['wr'])
            P.dma('sp', brb[:], I['b_router'].partition_broadcast(128), w=['brb'])
            h2f = [sbt('r_h2f%d' % i, [128, D], F32) for i in range(2)]
            h2Tf = [sbt('r_h2T%d' % i, [128, 8, 128], F32) for i in range(2)]
            xr = [sbt('r_xr%d' % i, [128, 4, XROW], BF16) for i in range(2)]
            tpf = [pst('r_tp%d' % i, [128, 8, 128], F32) for i in range(2)]
            lg_ps = [pst('r_lg%d' % i, [128, 512], F32) for i in range(2)]
            rk_ps = [pst('r_rk%d' % i, [128, 512], F32) for i in range(2)]
            lg = [sbt('r_lgs%d' % i, [128, NE], F32) for i in range(2)]
            mx8 = [sbt('r_mx%d' % i, [128, 8], F32) for i in range(2)]
            ix8 = [sbt('r_ix%d' % i, [128, 8], mybir.dt.uint32) for i in range(2)]
            ekf = [sbt('r_ek%d' % i, [128, 4], F32) for i in range(2)]
            e4 = [sbt('r_e4%d' % i, [128, 4], F32) for i in range(2)]
            p4 = [sbt('r_p4%d' % i, [128, 4], F32) for i in range(2)]
            sm = [sbt('r_sm%d' % i, [128, 4], F32) for i in range(2)]
            msk = [sbt('r_msk%d' % i, [128, NE], BF16) for i in range(2)]
            macc = sbt('r_macc', [128, NE], BF16)
            oh = [sbt('r_oh%d' % i, [128, NE], F32) for i in range(2)]
            rkk = [sbt('r_rkk%d' % i, [128, 4], F32) for i in range(2)]
            dstf = [sbt('r_dstf%d' % i, [128, 4], F32) for i in range(2)]
            ovf = [sbt('r_ovf%d' % i, [128, 4], F32) for i in range(2)]
            ovt = [sbt('r_ovt%d' % i, [128, 4], F32) for i in range(2)]
            P.dve(lambda e: e.memset(macc[:], 0.0), w=['macc'])
            tokf = [sbt('r_tokf%d' % i, [128, 1], F32) for i in range(2)]
            toki = [sbt('r_toki%d' % i, [128, 1], mybir.dt.int32) for i in range(2)]
            for i in range(2):
                P.dve(lambda e, i=i: e.memset(xr[i][:], 0.0), w=['xr%d' % i])
            iota = C.idf[:, 128:160]
            C.first_w = [loadW(I['w_e_gate'][0]), loadW(I['w_e_up'][0]), loadW(I['w_e_down'][0])]
            def tok_a(ti):
                b = ti % 2
                P.dma('sp', h2f[b][:], Dm['h2'][ti * 128:(ti + 1) * 128, :], w=['h2f%d' % b])
                for c in range(8):
                    P.pe(lambda e, b=b, c=c: e.transpose(tpf[b][:, c, :], h2f[b][:, c * 128:(c + 1) * 128], C.idf[:, 0:128]),
                         r=['h2f%d' % b, 'idf'], w=['tpf%d' % b])
                P.act(lambda e, b=b: e.copy(h2Tf[b][:], tpf[b][:]), w=['tpf%d' % b, 'h2Tf%d' % b])
                for k in range(8):
                    P.pe(lambda e, b=b, k=k: e.matmul(lg_ps[b][:, 0:NE], h2Tf[b][:, k, :], wr[:, k, :], start=(k == 0), stop=(k == 7)),
                         r=['h2Tf%d' % b, 'wr'], w=['lgps%d' % b])
                P.dve(lambda e, b=b: e.tensor_tensor(lg[b][:], lg_ps[b][:, 0:NE], brb[:], ALU.add), r=['brb'], w=['lgps%d' % b, 'lg%d' % b])
                P.dve(lambda e, b=b: e.max(mx8[b][:], lg[b][:]), r=['lg%d' % b], w=['mx8%d' % b])
                P.dve(lambda e, b=b: e.max_index(ix8[b][:], mx8[b][:], lg[b][:]), r=['lg%d' % b, 'mx8%d' % b], w=['ix8%d' % b])
                P.dve(lambda e, b=b: e.tensor_copy(ekf[b][:], ix8[b][:, 0:4]), r=['ix8%d' % b], w=['ekf%d' % b])
                P.dve(lambda e, b=b: e.tensor_scalar(msk[b][:], lg[b][:], mx8[b][:, 3:4], None, ALU.is_ge),
                      r=['lg%d' % b, 'mx8%d' % b], w=['msk%d' % b])
                P.dve(lambda e, b=b: e.tensor_scalar(sm[b][:, 0:1], mx8[b][:, 0:1], -1.0, None, ALU.mult), r=['mx8%d' % b], w=['nm%d' % b])
                P.act(lambda e, b=b: e.activation(e4[b][:], mx8[b][:, 0:4], AF.Exp, bias=sm[b][:, 0:1]), r=['mx8%d' % b, 'nm%d' % b], w=['e4%d' % b])
                P.dve(lambda e, b=b: e.reduce_sum(sm[b][:, 1:2], e4[b][:], axis=AX.X), r=['e4%d' % b], w=['sum%d' % b])
                P.dve(lambda e, b=b: e.reciprocal(sm[b][:, 2:3], sm[b][:, 1:2]), r=['sum%d' % b], w=['rs%d' % b])
                P.dve(lambda e, b=b: e.tensor_scalar(p4[b][:], e4[b][:], sm[b][:, 2:3], None, ALU.mult), r=['e4%d' % b, 'rs%d' % b], w=['p4%d' % b])
            def tok_b(ti):
                b = ti % 2
                P.pe(lambda e, b=b: e.matmul(rk_ps[b][:, 0:NE], C.cs('mlt'), msk[b][:], start=True, stop=False), r=['cst', 'msk%d' % b], w=['rkps%d' % b])
                P.pe(lambda e, b=b: e.matmul(rk_ps[b][:, 0:NE], C.cs('ones'), macc[:], start=False, stop=True), r=['cst', 'macc'], w=['rkps%d' % b])
                P.pool(lambda e, b=b: e.tensor_tensor(macc[:], macc[:], msk[b][:], ALU.add), r=['macc', 'msk%d' % b], w=['macc'])
                for k in range(4):
                    P.dve(lambda e, b=b, k=k: e.tensor_scalar(oh[b][:], iota, ekf[b][:, k:k + 1], None, ALU.is_equal),
                          r=['idf', 'ekf%d' % b], w=['oh%d' % b])
                    P.dve(lambda e, b=b: e.tensor_tensor(oh[b][:], oh[b][:], rk_ps[b][:, 0:NE], ALU.mult), r=['oh%d' % b], w=['oh%d' % b, 'rkps%d' % b])
                    P.dve(lambda e, b=b, k=k: e.reduce_sum(rkk[b][:, k:k + 1], oh[b][:], axis=AX.X), r=['oh%d' % b], w=['rkk%d%d' % (b, k)])
                rkt = ['rkk%d%d' % (b, k) for k in range(4)]
                P.dve(lambda e, b=b: e.scalar_tensor_tensor(dstf[b][:], ekf[b][:], float(CAP), rkk[b][:], ALU.mult, ALU.add),
                      r=rkt + ['ekf%d' % b], w=['dstf%d' % b])
                P.dve(lambda e, b=b: e.tensor_scalar(ovf[b][:], rkk[b][:], float(CAP) - 0.5, None, ALU.is_ge), r=rkt, w=['ovf%d' % b])
                P.dve(lambda e, b=b: e.tensor_scalar(ovt[b][:], dstf[b][:], -1.0, float(NSLOT), ALU.mult, ALU.add), r=['dstf%d' % b], w=['ovt%d' % b])
                P.dve(lambda e, b=b: e.tensor_tensor(ovt[b][:], ovt[b][:], ovf[b][:], ALU.mult), r=['ovt%d' % b, 'ovf%d' % b], w=['ovt%d' % b])
                P.dve(lambda e, b=b: e.tensor_tensor(dstf[b][:], dstf[b][:], ovt[b][:], ALU.add), r=['ovt%d' % b, 'dstf%d' % b], w=['dstf%d' % b])
                P.dve(lambda e, b=b, ti=ti: e.tensor_copy(desti[:, ti, :], dstf[b][:]), r=['dstf%d' % b], w=['desti%d' % ti])
                P.dve(lambda e, b=b, ti=ti: e.tensor_scalar(tokf[b][:], C.idf[:, 160:161], float(ti * 128), None, ALU.add),
                      r=['idf'], w=['tokf%d' % b])
                P.dve(lambda e, b=b: e.tensor_copy(toki[b][:], tokf[b][:]), r=['tokf%d' % b], w=['toki%d' % b])
                P.act(lambda e, b=b: e.copy(xr[b][:, 0, 0:D], h2f[b][:]), r=['h2f%d' % b], w=['xrx%d' % b], wa=['xr%d' % b])
                for k in range(1, 4):
                    P.dve(lambda e, b=b, k=k: e.tensor_copy(xr[b][:, k, 0:D], xr[b][:, 0, 0:D]), r=['xrx%d' % b], wa=['xr%d' % b])
                for k in range(4):
                    P.dve(lambda e, b=b, k=k: e.tensor_copy(xr[b][:, k, D:D + 2].bitcast(F32), p4[b][:, k:k + 1]),
                          r=['p4%d' % b], wa=['xr%d' % b])
                    P.dve(lambda e, b=b, k=k: e.tensor_copy(xr[b][:, k, D + 2:D + 4].bitcast(mybir.dt.int32), toki[b][:]),
                          r=['toki%d' % b], wa=['xr%d' % b])
                for k in range(4):
                    P.op('pool', lambda e, b=b, k=k, ti=ti: e.indirect_dma_start(
                        out=xrows_d[:, :], out_offset=bass.IndirectOffsetOnAxis(ap=desti[:, ti, k:k + 1], axis=0),
                        in_=xr[b][:, k, :], in_offset=None),
                        ['xr%d' % b, 'desti%d' % ti], ['xrows_%d_%d' % (ti, k)], dma=True)
            tok_a(0)
            for ti in range(NT):
                if ti + 1 < NT:
                    tok_a(ti + 1)
                tok_b(ti)
            P.flush()
        with ExitStack() as ex_:
            sbt = lambda n, s, d: C.sbt(ex_, n, s, d)
            pst = lambda n, s, d: C.pst(ex_, n, s, d)
            bd = [sbt('g_bd%d' % i, [1, D], BF16) for i in range(2)]
            xg = [sbt('g_xg%d' % i, [128, 4, XROW], BF16) for i in range(2)]
            xT = [sbt('g_xT%d' % i, [128, 8, 512], BF16) for i in range(2)]
            actT = [sbt('g_act%d' % i, [128, 8, 512], BF16) for i in range(2)]
            gc = [sbt('g_gc%d' % i, [128, 512], F32) for i in range(2)]
            sg = [sbt('g_sg%d' % i, [128, 512], F32) for i in range(2)]
            u1 = [sbt('g_u1%d' % i, [128, 512], F32) for i in range(2)]
            yw = [sbt('g_yw%d' % i, [128, D], F32) for i in range(2)]
            tp = [pst('g_tp%d' % i, [128, 8, 128], BF16) for i in range(2)]
            g_ps = [pst('g_gps%d' % i, [128, 512], F32) for i in range(2)]
            u_ps = [pst('g_ups%d' % i, [128, 512], F32) for i in range(2)]
            y_ps = [pst('g_yps%d' % i, [128, 512], F32) for i in range(2)]
            NG = CAP // 512
            groups = [(e_, gi) for e_ in range(NE) for gi in range(NG)]
            wsel = {0: tuple(C.first_w)}
            cnt = {'nf': 0, 'ny': 0, 'ntp': 0}

            def load_x(i):
                e_, gi = groups[i]
                base = e_ * CAP + gi * 512
                ab = i % 2
                P.dma('sp', xg[ab][:], xrows_d[base:base + 512, :].rearrange("(b p) c -> p b c", p=128), w=['xg%d' % ab])

            def phase_t(i):
                ab = i % 2
                for b4 in range(4):
                    tb = cnt['ntp'] % 2
                    cnt['ntp'] += 1
                    for c in range(8):
                        P.pe(lambda e, tb=tb, b4=b4, c=c: e.transpose(tp[tb][:, c, :], xg[ab][:, b4, c * 128:(c + 1) * 128], C.ident),
                             r=['xg%d' % ab], w=['tp%d' % tb])
                    if b4 % 2 == 0:
                        P.act(lambda e, tb=tb, b4=b4: e.copy(xT[ab][:, :, b4 * 128:(b4 + 1) * 128], tp[tb][:]), w=['tp%d' % tb], wa=['xT%d' % ab])
                    else:
                        P.dve(lambda e, tb=tb, b4=b4: e.tensor_copy(xT[ab][:, :, b4 * 128:(b4 + 1) * 128], tp[tb][:]), w=['tp%d' % tb], wa=['xT%d' % ab])

            def phase_gu(i):
                e_, gi = groups[i]
                ab = i % 2
                ig, iu, idn = wsel[e_]
                for fc in range(8):
                    fb = cnt['nf'] % 2
                    cnt['nf'] += 1
                    col = e_ * 8 + fc
                    for k in range(8):
                        P.pe(lambda e, fb=fb, k=k, fc=fc: e.matmul(
                            g_ps[fb][:], W[ig][:, k, fc * 128:(fc + 1) * 128], xT[ab][:, k, :], start=(k == 0), stop=(k == 7)),
                            r=['W%d' % ig, 'xT%d' % ab], w=['gps%d' % fb])
                    for k in range(8):
                        P.pe(lambda e, fb=fb, k=k, fc=fc: e.matmul(
                            u_ps[fb][:], W[iu][:, k, fc * 128:(fc + 1) * 128], xT[ab][:, k, :], start=(k == 0), stop=(k == 7)),
                            r=['W%d' % iu, 'xT%d' % ab], w=['ups%d' % fb])
                    P.dve(lambda e, fb=fb, col=col: e.tensor_scalar(gc[fb][:], g_ps[fb][:], bgc[:, col:col + 1], 7.0, ALU.add, ALU.min),
                          r=['g_bg'], w=['gps%d' % fb, 'gc%d' % fb])
                    P.act(lambda e, fb=fb: e.activation(sg[fb][:], gc[fb][:], AF.Sigmoid, scale=1.702), r=['gc%d' % fb], w=['sg%d' % fb])
                    P.dve(lambda e, fb=fb, col=col: e.tensor_scalar(u1[fb][:], u_ps[fb][:], buc[:, col:col + 1], 8.0, ALU.add, ALU.min),
                          r=['g_bu'], w=['ups%d' % fb, 'u1%d' % fb])
                    P.dve(lambda e, fb=fb: e.tensor_tensor(sg[fb][:], sg[fb][:], gc[fb][:], ALU.mult),
                          r=['sg%d' % fb, 'gc%d' % fb], w=['sg%d' % fb])
                    P.dve(lambda e, fb=fb, fc=fc: e.scalar_tensor_tensor(actT[ab][:, fc, :], u1[fb][:], -6.0, sg[fb][:], ALU.max, ALU.mult),
                          r=['u1%d' % fb, 'sg%d' % fb], w=['actT%d' % ab])
                    if gi == 0:
                        pumpW(1)

            def phase_d(i):
                e_, gi = groups[i]
                ab = i % 2
                bb = e_ % 2
                ig, iu, idn = wsel[e_]
                base = e_ * CAP + gi * 512
                for b4 in range(4):
                    wb_ = cnt['ny'] % 2
                    cnt['ny'] += 1
                    for hf in range(2):
                        for fc in range(8):
                            P.pe(lambda e, hf=hf, fc=fc, b4=b4: e.matmul(
                                y_ps[hf][:], actT[ab][:, fc, b4 * 128:(b4 + 1) * 128],
                                W[idn][:, fc, hf * 512:(hf + 1) * 512], start=(fc == 0), stop=False),
                                r=['actT%d' % ab, 'W%d' % idn], w=['yps%d' % hf])
                        P.pe(lambda e, hf=hf: e.matmul(
                            y_ps[hf][:], C.cs('ones')[0:1, :], bd[bb][0:1, hf * 512:(hf + 1) * 512],
                            start=False, stop=True), r=['cst', 'bd%d' % bb], w=['yps%d' % hf])
                        evac = P.act if hf == 0 else P.act
                        evac(lambda e, hf=hf, wb_=wb_, b4=b4: e.activation(
                            yw[wb_][:, hf * 512:(hf + 1) * 512], y_ps[hf][:], AF.Identity, scale=xg[ab][:, b4, D:D + 2].bitcast(F32)),
                            r=['xg%d' % ab], w=['yps%d' % hf], wa=['yw%d' % wb_])
                    P.op('pool', lambda e, wb_=wb_, b4=b4: e.indirect_dma_start(
                        out=C.ff_d[:, :], out_offset=bass.IndirectOffsetOnAxis(ap=xg[ab][:, b4, D + 2:D + 4].bitcast(mybir.dt.int32), axis=0),
                        in_=yw[wb_][:], in_offset=None, compute_op=ALU.add),
                        ['yw%d' % wb_, 'xg%d' % ab], ['ffacc'], dma=True)
                    if gi == NG - 1:
                        pumpW(1)

            load_x(0)
            phase_t(0)
            for i, (e_, gi) in enumerate(groups):
                if gi == 0:
                    bb = e_ % 2
                    P.dma('pool', bd[bb][:], I['b_e_down'][e_:e_ + 1, :], w=['bd%d' % bb])
                    if e_ + 1 < NE:
                        nxt_gu = (queueW(I['w_e_gate'][e_ + 1]), queueW(I['w_e_up'][e_ + 1]))
                if i + 1 < len(groups):
                    load_x(i + 1)
                phase_gu(i)
                if gi == NG - 1 and e_ + 1 < NE:
                    pumpW(8)
                    wsel[e_ + 1] = (nxt_gu[0], nxt_gu[1], queueW(I['w_e_down'][e_ + 1]))
                if i + 1 < len(groups):
                    phase_t(i + 1)
                phase_d(i)
            P.flush()
        with ExitStack() as fin:
            sbt = lambda n, s, d: C.sbt(fin, n, s, d)
            g_bc = sbt('h_g', [128, D], F32)
            b_bc = sbt('h_b', [128, D], F32)
            load_ln_params(C, I['ln3_g'], I['ln3_b'], g_bc, b_bc)
            ffl = [sbt('h_ff%d' % i, [128, D], F32) for i in range(2)]
            h2 = [sbt('h_h2%d' % i, [128, D], F32) for i in range(2)]
            res = [sbt('h_res%d' % i, [128, D], F32) for i in range(2)]
            ot = [sbt('h_ot%d' % i, [128, D], F32) for i in range(2)]
            stt = [sbt('h_st%d' % i, [128, 8], F32) for i in range(2)]
            junk = sbt('h_junk', [128, D], BF16)
            def h_p1(ti):
                b = ti % 2
                rs = slice(ti * 128, (ti + 1) * 128)
                P.dma('sp', ffl[b][:], C.ff_d[rs, :], w=['ffl%d' % b])
                P.dma('sp', h2[b][:], Dm['h2'][rs, :], w=['h2%d' % b])
                P.dve(lambda e, b=b: e.scalar_tensor_tensor(res[b][:], h2[b][:], ALPHA, ffl[b][:], ALU.mult, ALU.add),
                      r=['ffl%d' % b, 'h2%d' % b], w=['res%d' % b])
                ln_stats(C, res[b][:], 'res%d' % b, stt[b], junk[:], 'h%d' % b)

            h_p1(0)
            for ti in range(NT):
                b = ti % 2
                rs = slice(ti * 128, (ti + 1) * 128)
                if ti + 1 < NT:
                    h_p1(ti + 1)
                ln_apply(C, res[b][:], 'res%d' % b, ot[b][:], 'ot%d' % b, g_bc[:], b_bc[:], stt[b], 'h%d' % b)
                P.dma('pool', C.out[rs, :], ot[b][:], r=['ot%d' % b])
            P.flush()


_NC_CACHE = {}


def get_program(stop_after=None, debug=False):
    key = (stop_after, debug)
    if key not in _NC_CACHE:
        _NC_CACHE[key] = build_program(stop_after, debug)
    return _NC_CACHE[key]


def make_in_maps(inputs, n_cores=8):
    f = lambda a: np.ascontiguousarray(np.asarray(a, dtype=np.float32))
    shared = {
        'cst_bf': CST_BF, 'cst_f32': CST_F32, 'cst_i32': CST_I32,
        'ln_in_g': f(inputs['ln_in_g']), 'ln_in_b': f(inputs['ln_in_b']),
        'w_in': f(inputs['w_in'])[0], 'b_branch_gate': f(inputs['b_branch_gate'])[0].reshape(-1),
        'conv_w': f(inputs['conv_w'])[0], 'conv_b': f(inputs['conv_b'])[0],
        'dt_bias': f(inputs['dt_bias'])[0], 'a_log': f(inputs['a_log'])[0], 'd_skip': f(inputs['d_skip'])[0],
        'ssd_norm_g': f(inputs['ssd_norm_g'])[0], 'w_sb': f(inputs['w_sb'])[0], 'w_ssd': f(inputs['w_ssd'])[0],
        'w_mix_out': f(inputs['w_mix_out'])[0], 'ln1_g': f(inputs['ln1_g'])[0], 'ln1_b': f(inputs['ln1_b'])[0],
        'w_xq': f(inputs['w_xq'])[0], 'w_xk': f(inputs['w_xk'])[0], 'w_xv': f(inputs['w_xv'])[0],
        'w_xo': f(inputs['w_xo'])[0], 'ln2_g': f(inputs['ln2_g'])[0], 'ln2_b': f(inputs['ln2_b'])[0],
        'w_router': f(inputs['w_router'])[0], 'b_router': f(inputs['b_router'])[0],
        'w_e_gate': f(inputs['w_e_gate'])[0], 'b_e_gate': f(inputs['b_e_gate'])[0].reshape(-1),
        'w_e_up': f(inputs['w_e_up'])[0], 'b_e_up': f(inputs['b_e_up'])[0].reshape(-1),
        'w_e_down': f(inputs['w_e_down'])[0], 'b_e_down': f(inputs['b_e_down'])[0],
        'ln3_g': f(inputs['ln3_g'])[0], 'ln3_b': f(inputs['ln3_b'])[0],
    }
    x = f(inputs['x'])
    mem = f(inputs['mem'])
    maps = []
    for c in range(n_cores):
        m = dict(shared)
        m['x'] = x[c]
        m['mem'] = mem[c]
        maps.append(m)
    return maps


def kernel(**inputs):
    nc = get_program()
    in_maps = make_in_maps(inputs, 8)
    res = run_bass_kernel_spmd(nc, in_maps, core_ids=list(range(8)))
    return np.stack([np.asarray(r['out'], dtype=np.float32) for r in res.results], axis=0)
```
